# Optimizing a Trainium2 kernel written in Bass

```python
import jax, jax.numpy as jnp
from jax import lax
import numpy as np

D_MODEL = 1024
BATCH = 2
SEQ = 8192
DEPTH = 4

SWA_HEADS = 8
SWA_KV_HEADS = 2
SWA_HEAD_DIM = 64
SWA_WINDOW = 128
SWA_BLOCK = 128
ROPE_THETA = 10000.0
GLA_HEADS = 4
GLA_DK = D_MODEL // 2 // GLA_HEADS
GLA_DV = D_MODEL // GLA_HEADS
GLA_GATE_RANK = 16
GLA_TAU = 16.0
GLA_CHUNK = 64
D_FF = 2816
EPS = 1e-6

SWA_Q_W = SWA_HEADS * SWA_HEAD_DIM
SWA_KV_W = SWA_KV_HEADS * SWA_HEAD_DIM
GLA_K_W = GLA_HEADS * GLA_DK
GLA_V_W = GLA_HEADS * GLA_DV
IN_SPLITS = (SWA_Q_W, SWA_KV_W, SWA_KV_W, GLA_K_W, GLA_K_W, GLA_V_W, GLA_V_W, GLA_GATE_RANK, D_MODEL, D_MODEL)
IN_COLS = sum(IN_SPLITS)

kernel_name = "hybrid_swa_sink_gla_macaron"


def rmsnorm(x, g):
    xf = x.astype(jnp.float32)
    y = xf * lax.rsqrt(jnp.mean(xf * xf, axis=-1, keepdims=True) + EPS)
    return (y * g.astype(jnp.float32)).astype(x.dtype)


def swiglu(h, w_gate, w_up, w_down):
    return (jax.nn.silu(h @ w_gate) * (h @ w_up)) @ w_down


def split_cols(z, sizes):
    idx = np.cumsum(np.array(sizes))[:-1].tolist()
    return jnp.split(z, idx, axis=-1)


def rope_tables(T):
    inv_freq = ROPE_THETA ** (-jnp.arange(0, SWA_HEAD_DIM, 2, dtype=jnp.float32) / SWA_HEAD_DIM)
    ang = jnp.arange(T, dtype=jnp.float32)[:, None] * inv_freq[None, :]
    return jnp.cos(ang), jnp.sin(ang)


def apply_rope(x, cos, sin):
    x1, x2 = jnp.split(x, 2, axis=-1)
    c = cos[None, :, None, :]
    s = sin[None, :, None, :]
    return jnp.concatenate([x1 * c - x2 * s, x2 * c + x1 * s], axis=-1)


def swa_attention(q, k, v, q_gain, k_gain, sinks, cos, sin):
    B, T = q.shape[0], q.shape[1]
    G = SWA_HEADS // SWA_KV_HEADS
    nb = T // SWA_BLOCK
    q = apply_rope(rmsnorm(q, q_gain).astype(jnp.float32), cos, sin)
    k = apply_rope(rmsnorm(k, k_gain).astype(jnp.float32), cos, sin)
    v = v.astype(jnp.float32)
    q = q.reshape(B, nb, SWA_BLOCK, SWA_KV_HEADS, G, SWA_HEAD_DIM)
    k = k.reshape(B, nb, SWA_BLOCK, SWA_KV_HEADS, SWA_HEAD_DIM)
    v = v.reshape(B, nb, SWA_BLOCK, SWA_KV_HEADS, SWA_HEAD_DIM)

    def band(t):
        prev = jnp.pad(t[:, :-1], ((0, 0), (1, 0), (0, 0), (0, 0), (0, 0)))
        return jnp.concatenate([prev, t], axis=2)

    kb, vb = band(k), band(v)
    s = jnp.einsum('bnqhgd,bnkhd->bnhgqk', q, kb) * (SWA_HEAD_DIM ** -0.5)
    i = jnp.arange(SWA_BLOCK)[:, None]
    j = jnp.arange(2 * SWA_BLOCK)[None, :]
    rel = i + SWA_BLOCK - j
    blk = jnp.arange(nb)[:, None, None]
    valid = (rel >= 0) & (rel < SWA_WINDOW) & ((blk > 0) | (j >= SWA_BLOCK))
    s = jnp.where(valid[None, :, None, None], s, -jnp.inf)
    sink = sinks.astype(jnp.float32).reshape(1, 1, SWA_KV_HEADS, G, 1, 1)
    m = jnp.maximum(jnp.max(s, axis=-1, keepdims=True), sink)
    p = jnp.exp(s - m)
    denom = jnp.sum(p, axis=-1, keepdims=True) + jnp.exp(sink - m)
    o = jnp.einsum('bnhgqk,bnkhd->bnqhgd', p / denom, vb)
    return o.reshape(B, T, SWA_Q_W)


def gla_attention(q, k, v, log_a):
    B, T = q.shape[0], q.shape[1]
    nc = T // GLA_CHUNK

    def chunks(t, d):
        return t.astype(jnp.float32).reshape(B, nc, GLA_CHUNK, GLA_HEADS, d).transpose(1, 0, 3, 2, 4)

    qc = chunks(q, GLA_DK) * (GLA_DK ** -0.5)
    kc = chunks(k, GLA_DK)
    vc = chunks(v, GLA_DV)
    gc = chunks(log_a, GLA_DK)
    causal = jnp.tril(jnp.ones((GLA_CHUNK, GLA_CHUNK), dtype=bool))[:, :, None]

    def step(S, inp):
        qi, ki, vi, gi = inp
        b = jnp.cumsum(gi, axis=-2)
        b_last = b[:, :, -1:, :]
        o_inter = jnp.einsum('bhtc,bhcv->bhtv', qi * jnp.exp(b), S)
        diff = b[:, :, :, None, :] - b[:, :, None, :, :]
        decay = jnp.exp(jnp.where(causal, diff, -jnp.inf))
        attn = jnp.einsum('bhtsc,bhsc->bhts', qi[:, :, :, None, :] * decay, ki)
        o_intra = jnp.einsum('bhts,bhsv->bhtv', attn, vi)
        S = jnp.exp(b_last[:, :, 0, :])[..., None] * S + jnp.einsum('bhsc,bhsv->bhcv', ki * jnp.exp(b_last - b), vi)
        return S, o_inter + o_intra

    S0 = jnp.zeros((B, GLA_HEADS, GLA_DK, GLA_DV), jnp.float32)
    _, o = lax.scan(step, S0, (qc, kc, vc, gc))
    return o.transpose(1, 0, 3, 2, 4).reshape(B, T, GLA_HEADS, GLA_DV)


def setup_inputs(seed: int = 0) -> dict:
    key = jax.random.key(seed)
    ks = jax.random.split(key, 24)
    L, D = DEPTH, D_MODEL

    def w(k, shape, fan_in):
        return jax.random.normal(k, shape, jnp.float32) * (fan_in ** -0.5)

    def gain(k, shape):
        return 1.0 + 0.02 * jax.random.normal(k, shape, jnp.float32)

    return {
        "x": jax.random.normal(ks[0], (BATCH, SEQ, D), jnp.float32),
        "ffn1_norm": gain(ks[1], (L, D)),
        "ffn1_w_gate": w(ks[2], (L, D, D_FF), D),
        "ffn1_w_up": w(ks[3], (L, D, D_FF), D),
        "ffn1_w_down": w(ks[4], (L, D_FF, D), D_FF),
        "mix_norm": gain(ks[5], (L, D)),
        "w_in": w(ks[6], (L, D, IN_COLS), D),
        "swa_q_norm": gain(ks[7], (L, SWA_HEAD_DIM)),
        "swa_k_norm": gain(ks[8], (L, SWA_HEAD_DIM)),
        "swa_sinks": 0.5 * jax.random.normal(ks[9], (L, SWA_HEADS), jnp.float32),
        "gla_w_gate": w(ks[10], (L, GLA_GATE_RANK, GLA_K_W), GLA_GATE_RANK),
        "gla_gate_bias": 0.1 * jax.random.normal(ks[11], (L, GLA_K_W), jnp.float32),
        "gla_out_norm": gain(ks[12], (L, GLA_V_W)),
        "w_proj_a": w(ks[13], (L, SWA_Q_W, D), SWA_Q_W),
        "w_proj_b": w(ks[14], (L, GLA_V_W, D), GLA_V_W),
        "w_out": w(ks[15], (L, D, D), D),
        "ffn2_norm": gain(ks[16], (L, D)),
        "ffn2_w_gate": w(ks[17], (L, D, D_FF), D),
        "ffn2_w_up": w(ks[18], (L, D, D_FF), D),
        "ffn2_w_down": w(ks[19], (L, D_FF, D), D_FF),
    }


def reference(x, ffn1_norm, ffn1_w_gate, ffn1_w_up, ffn1_w_down, mix_norm, w_in,
              swa_q_norm, swa_k_norm, swa_sinks, gla_w_gate, gla_gate_bias, gla_out_norm,
              w_proj_a, w_proj_b, w_out, ffn2_norm, ffn2_w_gate, ffn2_w_up, ffn2_w_down):
    B, T = x.shape[0], x.shape[1]
    cos, sin = rope_tables(T)
    for l in range(DEPTH):
        h = rmsnorm(x, ffn1_norm[l])
        x = x + 0.5 * swiglu(h, ffn1_w_gate[l], ffn1_w_up[l], ffn1_w_down[l])

        h = rmsnorm(x, mix_norm[l])
        z = h @ w_in[l]
        q_a, k_a, v_a, q_b, k_b, v_b, r_b, g_lr, gate_a, gate_b = split_cols(z, IN_SPLITS)

        o_a = swa_attention(q_a.reshape(B, T, SWA_HEADS, SWA_HEAD_DIM),
                            k_a.reshape(B, T, SWA_KV_HEADS, SWA_HEAD_DIM),
                            v_a.reshape(B, T, SWA_KV_HEADS, SWA_HEAD_DIM),
                            swa_q_norm[l], swa_k_norm[l], swa_sinks[l], cos, sin).astype(x.dtype)

        log_a = jax.nn.log_sigmoid((g_lr @ gla_w_gate[l] + gla_gate_bias[l]).astype(jnp.float32)) / GLA_TAU
        o_b = gla_attention(q_b.reshape(B, T, GLA_HEADS, GLA_DK),
                            k_b.reshape(B, T, GLA_HEADS, GLA_DK),
                            v_b.reshape(B, T, GLA_HEADS, GLA_DV),
                            log_a.reshape(B, T, GLA_HEADS, GLA_DK))
        o_b = rmsnorm(o_b, gla_out_norm[l].reshape(GLA_HEADS, GLA_DV)).reshape(B, T, GLA_V_W).astype(x.dtype)
        o_b = o_b * jax.nn.silu(r_b)

        y_a = o_a @ w_proj_a[l]
        y_b = o_b @ w_proj_b[l]
        merged = jax.nn.sigmoid(gate_a) * y_a + jax.nn.sigmoid(gate_b) * y_b
        x = x + merged @ w_out[l]

        h = rmsnorm(x, ffn2_norm[l])
        x = x + 0.5 * swiglu(h, ffn2_w_gate[l], ffn2_w_up[l], ffn2_w_down[l])
    return x
```

```python
import contextlib
import numpy as np
import concourse.bass as bass
import concourse.mybir as mybir
from concourse.bass_utils import run_bass_kernel_spmd

F32 = mybir.dt.float32
BF16 = mybir.dt.bfloat16
AF = mybir.ActivationFunctionType
ALU = mybir.AluOpType

D = 1024
DFF = 2816
B_ = 2
SEQ = 8192
DEPTH = 4
EPS = 1e-6
INC = 5904
C_QA, C_KA, C_VA, C_QB, C_KB, C_VB, C_RB, C_GLR, C_GA, C_GB = 0, 512, 640, 768, 1280, 1792, 2816, 3840, 3856, 4880
NPCOL = 48
ENGS = ("pe", "act", "dve", "pool", "sp")


class Buf:
    __slots__ = ("name", "writer", "readers", "excl")

    def __init__(self, name, excl=False, after=()):
        self.name = name
        self.writer = None
        self.readers = []
        self.excl = excl
        if after:
            newest = {}
            dmas = {}
            for o in after:
                for a in ([o.writer] if o.writer is not None else []) + o.readers:
                    if a.dma:
                        dmas[id(a)] = a
                    elif a.eng not in newest or newest[a.eng].seq < a.seq:
                        newest[a.eng] = a
            self.readers = list(newest.values()) + list(dmas.values())


class Op:
    __slots__ = ("eng", "fn", "waits", "signal", "dma", "semkey", "semval", "seq")
    _n = 0

    def __init__(self, eng, fn, dma):
        Op._n += 1
        self.seq = Op._n
        self.eng = eng
        self.fn = fn
        self.dma = dma
        self.waits = []
        self.signal = False
        self.semkey = None
        self.semval = None


class Sched:
    def __init__(self, nc):
        self.nc = nc
        self.ops = {e: [] for e in ENGS}
        self.all_ops = []

    def op(self, eng, fn, reads=(), writes=(), dma=False, semkey=None):
        o = Op(eng, fn, dma)
        deps = []
        for b in reads:
            if b.writer is not None:
                deps.append(b.writer)
            if b.excl:
                deps.extend(b.readers)
        for b in writes:
            if b.writer is not None:
                deps.append(b.writer)
            deps.extend(b.readers)
        seen = set()
        for d in deps:
            if d is o or id(d) in seen:
                continue
            seen.add(id(d))
            if d.eng == "pe" and eng == "pe" and not d.dma and not dma:
                continue
            o.waits.append(d)
        for b in reads:
            if b.excl:
                b.writer = o
                b.readers = []
            else:
                if not dma:
                    b.readers = [r for r in b.readers if r.dma or r.eng != eng]
                b.readers.append(o)
        for b in writes:
            b.writer = o
            b.readers = []
        if dma:
            o.semkey = semkey if semkey is not None else writes[0]
        self.ops[eng].append(o)
        self.all_ops.append(o)
        return o

    def emit(self, final_wait_ops=()):
        nc = self.nc
        for o in self.all_ops:
            for d in o.waits:
                d.signal = True
        for o in final_wait_ops:
            o.signal = True
        for o in self.all_ops:
            if o.dma:
                o.signal = True
        with contextlib.ExitStack() as stack:
            eng_sem = {e: stack.enter_context(nc.semaphore("s_" + e)) for e in ENGS}
            dma_sem = {}
            cnt = {e: 0 for e in ENGS}
            dcount = {}
            for o in self.all_ops:
                if not o.signal:
                    continue
                if o.dma:
                    key = o.semkey
                    if key not in dma_sem:
                        dma_sem[key] = stack.enter_context(nc.semaphore("d_%d" % len(dma_sem)))
                        dcount[key] = 0
                    dcount[key] += 16
                    o.semval = (dma_sem[key], dcount[key])
                else:
                    cnt[o.eng] += 1
                    o.semval = (eng_sem[o.eng], cnt[o.eng])
            self.stats = dict(cnt=cnt, n_dma_sems=len(dma_sem), n_ops={e: len(self.ops[e]) for e in ENGS})
            block = stack.enter_context(nc.Block())
            engobj = {"pe": "tensor", "act": "scalar", "dve": "vector", "pool": "gpsimd", "sp": "sync"}

            def make(ename):
                ops = self.ops[ename]

                def body(eng):
                    known = {}
                    for o in ops:
                        need = {}
                        for d in o.waits:
                            s, v = d.semval
                            if need.get(s, 0) < v:
                                need[s] = v
                        for s, v in need.items():
                            if known.get(s, 0) >= v:
                                continue
                            eng.wait_ge(s, v)
                            known[s] = v
                        ins = o.fn(eng)
                        if o.signal:
                            s, v = o.semval
                            ins.then_inc(s, 16 if o.dma else 1)
                    if ename == "sp":
                        for o in final_wait_ops:
                            s, v = o.semval
                            eng.wait_ge(s, v)
                return body

            for e in ENGS:
                getattr(block, engobj[e])(make(e))


def run_interleaved(gens, width):
    it = iter(gens)
    active = []
    more = True
    while True:
        while more and len(active) < width:
            try:
                active.append(next(it))
            except StopIteration:
                more = False
        if not active:
            break
        for g in list(active):
            try:
                next(g)
            except StopIteration:
                active.remove(g)


class Prog:
    def __init__(self, T, nseg, nl, steps, stop_after=None, dbg=False):
        self.dbg = dbg
        self.T, self.NSEG, self.NL, self.steps = T, nseg, nl, steps
        self.NQ, self.NT = T // 512, T // 128
        self.stop_after = stop_after
        nc = self.nc = bass.Bass("TRN2", target_bir_lowering=False)
        self.S = Sched(nc)
        self.stack = contextlib.ExitStack()
        self.outs = []
        self.ps_rr = 0

    def din(self, name, shape, dt=F32):
        return self.nc.dram_tensor(name, list(shape), dt, kind="ExternalInput").ap()

    def dout(self, name, shape, dt=F32):
        return self.nc.dram_tensor(name, list(shape), dt, kind="ExternalOutput").ap()

    def sb(self, name, shape, dt):
        t = self.stack.enter_context(self.nc.sbuf_tensor("sb_" + name, list(shape), dt))
        return t

    def op(self, *a, **k):
        return self.S.op(*a, **k)

    def psum(self):
        i = self.ps_rr
        self.ps_rr = (i + 1) % 8
        return self.ps_t[i], self.ps_b[i]

    def build(self):
        with self.stack:
            self._alloc()
            self._consts()
            for (sg, l) in self.steps:
                self._seglayer(sg, l)
            self.S.emit(final_wait_ops=self.outs)
        return self.nc

    def _alloc(self):
        T, NL, NSEG = self.T, self.NL, self.NSEG
        d = self.dram = {}
        d["xin"] = self.din("xin", [NSEG, D, T])
        d["xout"] = self.dout("xout", [NSEG, D, T])
        d["rope"] = self.din("rope", [NSEG, 2, 128, T])
        d["st_in"] = self.din("st_in", [NL, 4, 128, 256])
        d["st_out"] = self.dout("st_out", [NL, 4, 128, 256])
        d["kh_in"] = self.din("kh_in", [NL, 2, 128, 128])
        d["kh_out"] = self.dout("kh_out", [NL, 2, 128, 128])
        d["vh_in"] = self.din("vh_in", [NL, 128, 130])
        d["vh_out"] = self.dout("vh_out", [NL, 128, 130])
        d["cmat"] = self.din("cmat", [128, 6 * 128])
        d["pcol"] = self.din("pcol", [NL, 128, NPCOL])
        d["prow"] = self.din("prow", [NL, 1, 128])
        for n, shp in [("ffn1_w_gate", [D, DFF]), ("ffn1_w_up", [D, DFF]), ("ffn1_w_down", [DFF, D]),
                       ("ffn2_w_gate", [D, DFF]), ("ffn2_w_up", [D, DFF]), ("ffn2_w_down", [DFF, D]),
                       ("w_in", [D, INC]), ("gla_w_gate", [16, 512]), ("w_proj_a", [512, D]),
                       ("w_proj_b", [D, D]), ("w_out", [D, D])]:
            d[n] = self.din(n, [NL] + shp)

        sb = self.sb
        self.xT = sb("xT", [128, 8, T], F32)
        self.hT = sb("hT", [128, 8, T], BF16)
        self.xb = [[Buf("x%d_%d" % (k, q)) for q in range(self.NQ)] for k in range(8)]
        self.hb = [Buf("h%d" % q) for q in range(self.NQ)]
        self.NSLOT = 6
        self.wslot = [sb("w%d" % i, [128, 4096], BF16) for i in range(self.NSLOT)]
        self.wbuf = [Buf("w%d" % i) for i in range(self.NSLOT)]
        self.w_rr = 0
        self.stg_rr = 0
        self.AR = 45056
        self.arena = sb("arena", [128, self.AR], BF16)
        self.arena_hist = []
        self.ps_t = [self.stack.enter_context(self.nc.psum_tensor("ps%d" % i, [128, 512], F32)) for i in range(8)]
        self.ps_b = [Buf("ps%d" % i, excl=True) for i in range(8)]
        self.cm = sb("cm", [128, 6 * 128], BF16)
        self.cmb = Buf("cm")
        self.onesf = sb("onesf", [128, 512], F32)
        self.onesb = Buf("onesf")
        self.pcol = sb("pcol", [128, NPCOL], F32)
        self.pcolb = Buf("pcol")
        self.prow = sb("prow", [1, 128], F32)
        self.prowb = Buf("prow")
        self.small = sb("small", [128, 64], F32)
        self.smallb = Buf("small")
        self.wgate = sb("wgate", [16, 512], BF16)
        self.wgateb = Buf("wgate")
        self.rope = sb("ropet", [128, 2, T], F32)
        self.ropeb = Buf("rope")
        self.Sst = sb("Sst", [128, 4, 256], F32)
        self.Sbf = sb("Sbf", [128, 4, 256], BF16)
        self.Sb = [Buf("S%d" % h) for h in range(4)]
        self.Sbfb = [Buf("Sbf%d" % h) for h in range(4)]
        self.tiny = sb("tiny", [1, 256], F32)
        self.tinyb = Buf("tiny")
        self.stb = [Buf("st%d" % i) for i in range(NL)]
        self.khb = [Buf("kh%d" % i) for i in range(NL)]
        self.vhb = [Buf("vh%d" % i) for i in range(NL)]
        if self.dbg:
            d["dbg_oa"] = self.dout("dbg_oa", [4, 128, T], BF16)
            d["dbg_ob"] = self.dout("dbg_ob", [8, 128, T], BF16)
        self.cur_x = None

    def acompact(self):
        if len(self.arena_hist) > 1:
            f = Buf("fence", after=[hb for (_, _, hb) in self.arena_hist])
            self.arena_hist = [(0, self.AR, f)]

    def aview(self, off, n, dt, name):
        nb = 2 * n if dt == F32 else n
        assert off + nb <= self.AR, (name, off, nb, self.AR)
        if dt == F32:
            assert off % 2 == 0
            v = self.arena[:, off:off + nb].bitcast(F32)
        else:
            v = self.arena[:, off:off + nb]
        b = Buf(name, after=[hb for (s0, e0, hb) in self.arena_hist if s0 < off + nb and off < e0])
        self.arena_hist.append((off, off + nb, b))
        return v, b

    def _consts(self):
        cm, d = self.cm, self.dram
        self.op("pool", lambda e: e.dma_start(out=cm[:], in_=d["cmat"]), writes=[self.cmb], dma=True)
        self.op("pool", lambda e: e.memset(self.onesf[:], 1.0), writes=[self.onesb])
        self.ident = cm[:, 0:128]
        self.ones = cm[:, 128:256]
        self.blk = cm[:, 256:384]
        self.rot = cm[:, 384:512]
        self.mcur = cm[:, 512:640]
        self.mprev = cm[:, 640:768]

    def wload(self, srcs, slot):
        t, b = self.wslot[slot], self.wbuf[slot]
        for dv, src in srcs:
            self.op("pool", lambda e, dv=dv, src=src, t=t: e.dma_start(out=dv(t), in_=src), writes=[b], dma=True)
        return t, b

    def wcols(self, wname, l, c0, w, slot):
        src = self.dram[wname][l, :, c0:c0 + w].rearrange("(k p) c -> p k c", p=128)
        t, b = self.wload([(lambda t, w=w: t[:, 0:8 * w].rearrange("p (k c) -> p k c", c=w), src)], slot)
        return t[:, 0:8 * w].rearrange("p (k c) -> p k c", c=w), b

    def wrows(self, wname, l, r0, nj, ncol, slot):
        src = self.dram[wname][l, r0:r0 + nj * 128, 0:ncol].rearrange("(j p) c -> p j c", p=128)
        t, b = self.wload([(lambda t: t[:, 0:nj * ncol].rearrange("p (j c) -> p j c", c=ncol), src)], slot)
        return t[:, 0:nj * ncol].rearrange("p (j c) -> p j c", c=ncol), b

    def rmsnorm_h(self, gcol):
        T, NQ = self.T, self.NQ
        self.acompact()
        sq, sqb = zip(*[self.aview(i * 512, 512, BF16, "sq%d" % i) for i in range(4)])
        rstd, rstdb = self.aview(2048, 512, F32, "rstd")
        n = 0
        for q in range(NQ):
            qs = slice(q * 512, (q + 1) * 512)
            pt, pb = self.psum()
            for k in range(8):
                s, sb_ = sq[n % 4], sqb[n % 4]
                n += 1
                self.op("pool" if n % 3 == 0 else "dve", lambda e, s=s, k=k, qs=qs: e.tensor_tensor(out=s, in0=self.xT[:, k, qs], in1=self.xT[:, k, qs], op=ALU.mult),
                        reads=[self.xb[k][q]], writes=[sb_])
                self.op("pe", lambda e, s=s, k=k, pt=pt: e.matmul(pt[:], lhsT=self.ones, rhs=s, start=(k == 0), stop=(k == 7)),
                        reads=[sb_, self.cmb], writes=[pb])
            self.op("act", lambda e, pt=pt: e.activation(out=rstd, in_=pt[:], func=AF.Ln, scale=1.0 / D, bias=EPS), reads=[pb], writes=[rstdb])
            self.op("act", lambda e: e.activation(out=rstd, in_=rstd, func=AF.Exp, scale=-0.5), reads=[rstdb], writes=[rstdb])
            for k in range(8):
                self.op("dve", lambda e, k=k, qs=qs: e.scalar_tensor_tensor(out=self.hT[:, k, qs], in0=self.xT[:, k, qs], scalar=self.pcol[:, gcol + k:gcol + k + 1],
                                                                          in1=rstd, op0=ALU.mult, op1=ALU.mult),
                        reads=[self.xb[k][q], rstdb, self.pcolb], writes=[self.hb[q]])

    STG_OFF = 10240

    def wstaged(self, src, slot):
        i = self.stg_rr
        self.stg_rr = (i + 1) % 3
        sv, sbuf_ = self.aview(self.STG_OFF + i * 8192, 4096, F32, "stg%d" % i)
        t, b = self.wslot[slot], self.wbuf[slot]
        shp = src.shape
        n = shp[1] * shp[2]
        sv3 = sv[:, 0:n].rearrange("p (a c) -> p a c", c=shp[2])
        tv3 = t[:, 0:n].rearrange("p (a c) -> p a c", c=shp[2])
        self.op("sp", lambda e: e.dma_start(out=sv3, in_=src), writes=[sbuf_], dma=True, semkey="stg%d" % i)
        self.op("act", lambda e: e.activation(out=t[:, 0:n], in_=sv[:, 0:n], func=AF.Copy), reads=[sbuf_], writes=[b])
        return tv3, b

    def wcols_s(self, wname, l, c0, w, slot):
        return self.wstaged(self.dram[wname][l, :, c0:c0 + w].rearrange("(k p) c -> p k c", p=128), slot)

    def wrows_s(self, wname, l, r0, nj, ncol, slot):
        return self.wstaged(self.dram[wname][l, r0:r0 + nj * 128, 0:ncol].rearrange("(j p) c -> p j c", p=128), slot)

    def ffn_prefetch(self, l, which, staged=False):
        f = self.wcols_s if staged else self.wcols
        wg, wgb = f(which + "_w_gate", l, 0, 512, 0)
        wu, wub = f(which + "_w_up", l, 0, 512, 2)
        return (wg, wgb, wu, wub)

    def ffn(self, l, which, gcol, pre=None, post=None):
        T, NQ = self.T, self.NQ
        if pre is None:
            pre = self.ffn_prefetch(l, which, staged=True)
        self.rmsnorm_h(gcol)
        wg_n, wu_n, wd_n = which + "_w_gate", which + "_w_up", which + "_w_down"
        groups = [(g * 512, 512) for g in range(5)] + [(2560, 256)]
        aT = [self.arena[:, i * 4 * T:(i + 1) * 4 * T].rearrange("p (j t) -> p j t", t=T) for i in range(2)]
        aTb = [[[self.aview(i * 4 * T + j * T + q * 512, 512, BF16, "a%d_%d_%d" % (i, j, q))[1] for q in range(NQ)] for j in range(4)] for i in range(2)]
        sg, sgb = zip(*[self.aview(8 * T + i * 1024, 512, F32, "sg%d" % i) for i in range(2)])
        nsg = 0

        def down(gi, f0, fw, wd, wdb):
            nj = fw // 128
            a, ab = aT[gi % 2], aTb[gi % 2]
            for dc in range(8):
                for q in range(NQ):
                    qs = slice(q * 512, (q + 1) * 512)
                    pt, pb = self.psum()
                    for j in range(nj):
                        self.op("pe", lambda e, pt=pt, j=j, dc=dc, qs=qs, a=a, wd=wd: e.matmul(pt[:], lhsT=wd[:, j, dc * 128:(dc + 1) * 128], rhs=a[:, j, qs],
                                                                                               start=(j == 0), stop=(j == nj - 1)),
                                reads=[wdb, ab[j][q]], writes=[pb])
                    self.op("dve", lambda e, pt=pt, dc=dc, qs=qs: e.scalar_tensor_tensor(out=self.xT[:, dc, qs], in0=pt[:], scalar=0.5, in1=self.xT[:, dc, qs],
                                                                                         op0=ALU.mult, op1=ALU.add),
                            reads=[pb, self.xb[dc][q]], writes=[self.xb[dc][q]])

        pending = None
        for gi, (f0, fw) in enumerate(groups):
            nj = fw // 128
            if gi == 0:
                wg, wgb, wu, wub = pre
            else:
                wg, wgb = self.wcols_s(wg_n, l, f0, fw, gi % 2)
                wu, wub = self.wcols_s(wu_n, l, f0, fw, 2 + gi % 2)
            wd, wdb = self.wrows_s(wd_n, l, f0, nj, D, 4 + gi % 2)
            a, ab = aT[gi % 2], aTb[gi % 2]
            for j in range(nj):
                for q in range(NQ):
                    qs = slice(q * 512, (q + 1) * 512)
                    pg, pgb = self.psum()
                    pu, pub = self.psum()
                    for k in range(8):
                        self.op("pe", lambda e, pg=pg, k=k, j=j, qs=qs, wg=wg: e.matmul(pg[:], lhsT=wg[:, k, j * 128:(j + 1) * 128], rhs=self.hT[:, k, qs],
                                                                                        start=(k == 0), stop=(k == 7)),
                                reads=[wgb, self.hb[q]], writes=[pgb])
                    for k in range(8):
                        self.op("pe", lambda e, pu=pu, k=k, j=j, qs=qs, wu=wu: e.matmul(pu[:], lhsT=wu[:, k, j * 128:(j + 1) * 128], rhs=self.hT[:, k, qs],
                                                                                        start=(k == 0), stop=(k == 7)),
                                reads=[wub, self.hb[q]], writes=[pub])
                    s_, sb_ = sg[nsg % 2], sgb[nsg % 2]
                    nsg += 1
                    self.op("act", lambda e, pg=pg, s_=s_: e.activation(out=s_, in_=pg[:], func=AF.Silu), reads=[pgb], writes=[sb_])
                    self.op("dve", lambda e, pu=pu, s_=s_, a=a, j=j, qs=qs: e.tensor_tensor(out=a[:, j, qs], in0=pu[:], in1=s_, op=ALU.mult),
                            reads=[pub, sb_], writes=[ab[j][q]])
            if pending is not None:
                down(*pending)
            pending = (gi, f0, fw, wd, wdb)
        if post is not None:
            post()
        down(*pending)

    def _seglayer(self, sg, l):
        T, NQ, NT, d = self.T, self.NQ, self.NT, self.dram
        if self.cur_x != sg:
            for k in range(8):
                self.op("sp", lambda e, k=k: e.dma_start(out=self.xT[:, k, :], in_=d["xin"][sg, k * 128:(k + 1) * 128, :]),
                        writes=[self.xb[k][q] for q in range(NQ)], dma=True)
            self.op("sp", lambda e: e.dma_start(out=self.rope[:], in_=d["rope"][sg].rearrange("c p t -> p c t")), writes=[self.ropeb], dma=True)
            self.cur_x = sg
        self.op("sp", lambda e: e.dma_start(out=self.pcol[:], in_=d["pcol"][l]), writes=[self.pcolb], dma=True)
        self.op("sp", lambda e: e.dma_start(out=self.prow[:], in_=d["prow"][l]), writes=[self.prowb], dma=True)
        self.op("pool", lambda e: e.dma_start(out=self.wgate[:], in_=d["gla_w_gate"][l]), writes=[self.wgateb], dma=True)

        pre1 = getattr(self, "pre_next", None)
        self.pre_next = None
        self.ffn(l, "ffn1", 0, pre=pre1)
        if self.stop_after != "ffn1":
            self.pre2 = None
            self.mixer(sg, l)
            if self.stop_after != "mixer":
                idx = self.steps.index((sg, l))
                nxt = self.steps[idx + 1] if idx + 1 < len(self.steps) else None

                def post():
                    if nxt is not None:
                        self.pre_next = self.ffn_prefetch(nxt[1], "ffn1", staged=True)
                self.ffn(l, "ffn2", 16, pre=self.pre2, post=post)
        last_layer_for_seg = not any((s2 == sg and l2 > l) for (s2, l2) in self.steps)
        if last_layer_for_seg:
            for k in range(8):
                o = self.op("sp", lambda e, k=k: e.dma_start(out=d["xout"][sg, k * 128:(k + 1) * 128, :], in_=self.xT[:, k, :]),
                            reads=[self.xb[k][q] for q in range(NQ)], writes=[Buf("xo")], dma=True, semkey="xo%d" % k)
                self.outs.append(o)
            self.cur_x = None

    def mixer(self, sg, l):
        T, NQ, NT, d = self.T, self.NQ, self.NT, self.dram
        op, pcol, pcolb, hT, hb, xT = self.op, self.pcol, self.pcolb, self.hT, self.hb, self.xT
        cmb, sm, smb = self.cmb, self.small, self.smallb
        first = not any((l2 == l) for (s2, l2) in self.steps[:self.steps.index((sg, l))])
        wqa, wqab = self.wcols("w_in", l, C_QA, 512, 0)
        srcs = []
        for kv in range(2):
            src = d["w_in"][l, :, C_KA + kv * 64:C_KA + (kv + 1) * 64].rearrange("(k p) c -> p k c", p=128)
            for dup in range(2):
                c0 = kv * 128 + dup * 64
                srcs.append((lambda t, c0=c0: t[:, 0:2048].rearrange("p (k c) -> p k c", c=256)[:, :, c0:c0 + 64], src))
        srcs.append((lambda t: t[:, 2048:3072].rearrange("p (k c) -> p k c", c=128),
                     d["w_in"][l, :, C_VA:C_VA + 128].rearrange("(k p) c -> p k c", p=128)))
        tkv, wkvb = self.wload(srcs, 1)
        wka = tkv[:, 0:2048].rearrange("p (k c) -> p k c", c=256)
        wva = tkv[:, 2048:3072].rearrange("p (k c) -> p k c", c=128)
        wglr, wglrb = self.wcols("w_in", l, C_GLR, 16, 2)
        self.rmsnorm_h(8)

        off = [0]

        def al(n, dt, name):
            if dt == F32 and off[0] % 2:
                off[0] += 1
            v, b = self.aview(off[0], n, dt, name)
            off[0] += 2 * n if dt == F32 else n
            return v, b

        def al2(n, dt, name):
            r = [al(n, dt, "%s%d" % (name, i)) for i in range(2)]
            return [x[0] for x in r], [x[1] for x in r]

        oaT, _ = al(4 * T, BF16, "oaT")
        oaT3 = oaT.rearrange("p (j t) -> p j t", t=T)
        oab = [Buf("oa%d" % q) for q in range(NQ)]
        obT, _ = al(8 * T, BF16, "obT")
        obT3 = obT.rearrange("p (j t) -> p j t", t=T)
        obb = [[Buf("ob%d_%d" % (j, q)) for q in range(NQ)] for j in range(8)]
        base = off[0]
        for bb in oab:
            self.arena_hist.append((0, 4 * T, bb))
        for row in obb:
            for bb in row:
                self.arena_hist.append((4 * T, 12 * T, bb))

        tiny, tinyb = self.tiny, self.tinyb
        op("dve", lambda e: e.tensor_tensor(out=tiny[0:1, 0:128], in0=self.prow[0:1, :], in1=self.prow[0:1, :], op=ALU.mult), reads=[self.prowb], writes=[tinyb])
        op("dve", lambda e: e.reduce_max(out=tiny[0:1, 128:130], in_=tiny[0:1, 0:128].rearrange("p (a b) -> p a b", b=64), axis=mybir.AxisListType.X),
           reads=[tinyb], writes=[tinyb])
        op("dve", lambda e: e.tensor_tensor(out=tiny[0:1, 130:131], in0=tiny[0:1, 128:129], in1=tiny[0:1, 129:130], op=ALU.mult), reads=[tinyb], writes=[tinyb])
        pt, pb = self.psum()
        op("pe", lambda e, pt=pt: e.matmul(pt[:, 0:1], lhsT=self.onesf[0:1, 0:128], rhs=tiny[0:1, 130:131], start=True, stop=True), reads=[tinyb, self.onesb], writes=[pb])
        op("act", lambda e, pt=pt: e.activation(out=sm[:, 1:2], in_=pt[:, 0:1], func=AF.Ln), reads=[pb], writes=[smb])
        op("act", lambda e: e.activation(out=sm[:, 2:3], in_=sm[:, 1:2], func=AF.Exp, scale=0.5), reads=[smb], writes=[smb])
        op("dve", lambda e: e.tensor_scalar(out=sm[:, 0:1], in0=sm[:, 2:3], scalar1=-8.0, scalar2=None, op0=ALU.mult), reads=[smb], writes=[smb])
        op("act", lambda e: e.activation(out=sm[:, 8:16], in_=pcol[:, 38:46], func=AF.Exp, bias=sm[:, 0:1], scale=1.0), reads=[smb, pcolb], writes=[smb])
        op("dve", lambda e: e.tensor_scalar(out=sm[:, 20:24], in0=pcol[:, 34:38], scalar1=-1.0, scalar2=None, op0=ALU.mult), reads=[pcolb, smb], writes=[smb])
        negsmax = sm[:, 0:1]
        sinkexp = sm[:, 8:16]

        off[0] = base
        qaT, _ = al(4 * T, BF16, "qaT")
        qaT3 = qaT.rearrange("p (j t) -> p j t", t=T)
        qab = [[Buf("qa%d_%d" % (j, q)) for q in range(NQ)] for j in range(4)]
        kaT, _ = al(2 * (T + 128), BF16, "kaT")
        kaT3 = kaT.rearrange("p (v t) -> p v t", t=T + 128)
        kab = [[Buf("ka%d_%d" % (kv, q)) for q in range(NQ + 1)] for kv in range(2)]
        vaug, _ = al((NT + 1) * 130, BF16, "vaug")
        vaug4 = vaug.rearrange("p (n v e) -> p n v e", v=2, e=65)
        vab = [Buf("va%d" % n) for n in range(NT + 1)]
        for row in qab:
            for bb in row:
                self.arena_hist.append((base, base + 4 * T, bb))
        for row in kab:
            for bb in row:
                self.arena_hist.append((base + 4 * T, base + 4 * T + 2 * (T + 128), bb))
        for bb in vab:
            self.arena_hist.append((base + 4 * T + 2 * (T + 128), base + 4 * T + 2 * (T + 128) + (NT + 1) * 130, bb))
        mask4, mask4b = al(512, BF16, "mask4")
        def aln(n, dt, name, cnt_):
            r = [al(n, dt, "%s%d" % (name, i)) for i in range(cnt_)]
            return [x[0] for x in r], [x[1] for x in r]
        QW = 3
        qk_tmp_off = off[0]
        qraw, qrawb = aln(512, F32, "qraw", QW)
        sq, sqb = aln(512, BF16, "sqq", QW)
        rstd, rstdb = aln(512, F32, "rstdq", QW)
        qn, qnb = aln(512, BF16, "qn", QW)
        t1, t1b = aln(512, F32, "t1", QW)
        t2, t2b = aln(512, F32, "t2", QW)
        BW = 4
        off_after_qk = off[0]
        off[0] = qk_tmp_off
        Pt = [aln(512, BF16, "P%d_" % par, 2 * BW) for par in range(2)]
        oat, oatb = aln(512, BF16, "oat", BW)
        den, denb = aln(8, F32, "den", BW)
        off[0] = max(off[0], off_after_qk)

        for c in range(4):
            src = self.mprev if c < 2 else self.mcur
            op("pool", lambda e, c=c, src=src: e.tensor_copy(out=mask4[:, c * 128:(c + 1) * 128], in_=src), reads=[cmb], writes=[mask4b])

        ksrc = d["kh_in"] if first else d["kh_out"]
        vsrc = d["vh_in"] if first else d["vh_out"]
        op("pool", lambda e: e.dma_start(out=kaT3[:, :, 0:128], in_=ksrc[l].rearrange("v p t -> p v t")), reads=[self.khb[l]], writes=[kab[0][0], kab[1][0]], dma=True, semkey="khalo")
        op("pool", lambda e: e.dma_start(out=vaug[:, 0:130], in_=vsrc[l]), reads=[self.vhb[l]], writes=[vab[0]], dma=True, semkey="vhalo")
        op("pool", lambda e: e.memset(vaug4[:, 1:, :, 64:65], 1.0), writes=vab[1:])


        cnt = [0]

        def qk_chunk(lhsT_fn, wb, gaincol, dst, dstb, q):
            i = cnt[0] % QW
            cnt[0] += 1
            qs = slice(q * 512, (q + 1) * 512)
            pt, pb = self.psum()
            for k in range(8):
                op("pe", lambda e, pt=pt, k=k: e.matmul(pt[:], lhsT=lhsT_fn(k), rhs=hT[:, k, qs], start=(k == 0), stop=(k == 7)), reads=[wb, hb[q]], writes=[pb])
            op("act", lambda e, pt=pt: e.activation(out=qraw[i], in_=pt[:], func=AF.Copy), reads=[pb], writes=[qrawb[i]])
            op("pool", lambda e: e.tensor_tensor(out=sq[i], in0=qraw[i], in1=qraw[i], op=ALU.mult), reads=[qrawb[i]], writes=[sqb[i]])
            yield
            p2, p2b = self.psum()
            op("pe", lambda e, p2=p2: e.matmul(p2[:], lhsT=self.blk, rhs=sq[i], start=True, stop=True), reads=[sqb[i], cmb], writes=[p2b])
            op("act", lambda e, p2=p2: e.activation(out=rstd[i], in_=p2[:], func=AF.Ln, scale=1.0 / 64, bias=EPS), reads=[p2b], writes=[rstdb[i]])
            op("act", lambda e: e.activation(out=rstd[i], in_=rstd[i], func=AF.Exp, scale=-0.5), reads=[rstdb[i]], writes=[rstdb[i]])
            op("dve", lambda e: e.scalar_tensor_tensor(out=qn[i], in0=qraw[i], scalar=pcol[:, gaincol:gaincol + 1], in1=rstd[i], op0=ALU.mult, op1=ALU.mult),
               reads=[qrawb[i], rstdb[i], pcolb], writes=[qnb[i]])
            yield
            p3, p3b = self.psum()
            op("pe", lambda e, p3=p3: e.matmul(p3[:], lhsT=self.rot, rhs=qn[i], start=True, stop=True), reads=[qnb[i], cmb], writes=[p3b])
            op("dve", lambda e, p3=p3: e.tensor_tensor(out=t2[i], in0=p3[:], in1=self.rope[:, 1, qs], op=ALU.mult), reads=[p3b, self.ropeb], writes=[t2b[i]])
            op("pool", lambda e: e.tensor_tensor(out=t1[i], in0=qn[i], in1=self.rope[:, 0, qs], op=ALU.mult), reads=[qnb[i], self.ropeb], writes=[t1b[i]])
            op("pool", lambda e: e.tensor_tensor(out=dst, in0=t1[i], in1=t2[i], op=ALU.add), reads=[t1b[i], t2b[i]], writes=[dstb])

        def qk_gens():
            for q in range(NQ):
                qs = slice(q * 512, (q + 1) * 512)
                for kv in range(2):
                    yield qk_chunk(lambda k, kv=kv: wka[:, k, kv * 128:(kv + 1) * 128], wkvb, 33, kaT3[:, kv, 128 + q * 512:128 + (q + 1) * 512], kab[kv][q + 1], q)
                for j in range(4):
                    yield qk_chunk(lambda k, j=j: wqa[:, k, j * 128:(j + 1) * 128], wqab, 32, qaT3[:, j, qs], qab[j][q], q)
        run_interleaved(qk_gens(), QW)
        for n in range(NT):
            pt, pb = self.psum()
            for k in range(8):
                op("pe", lambda e, pt=pt, k=k, n=n: e.matmul(pt[:, 0:128], lhsT=hT[:, k, n * 128:(n + 1) * 128], rhs=wva[:, k, :], start=(k == 0), stop=(k == 7)),
                   reads=[hb[n // 4], wkvb], writes=[pb])
            op("act", lambda e, pt=pt, n=n: e.activation(out=vaug4[:, n + 1, :, 0:64], in_=pt[:, 0:128].rearrange("p (a b) -> p a b", b=64), func=AF.Copy),
               reads=[pb], writes=[vab[n + 1]])

        o1 = op("pool", lambda e: e.dma_start(out=d["kh_out"][l].rearrange("v p t -> p v t"), in_=kaT3[:, :, T:T + 128]),
                reads=[kab[0][NQ], kab[1][NQ], kab[0][0], kab[1][0]], writes=[self.khb[l]], dma=True)
        o2 = op("pool", lambda e: e.dma_start(out=d["vh_out"][l], in_=vaug[:, NT * 130:(NT + 1) * 130]), reads=[vab[NT], vab[0]], writes=[self.vhb[l]], dma=True)
        self.outs += [o1, o2]

        def swa_block(i, slot):
            q = i // 4
            for kv in range(2):
                Pv = [Pt[par][0][slot * 2 + kv] for par in range(2)]
                Pb = [Pt[par][1][slot * 2 + kv] for par in range(2)]
                for par in range(2):
                    bank, bankb = self.psum()
                    ps_ = slice(par * 64, (par + 1) * 64)
                    for kt in range(2):
                        kq = (i * 128 + kt * 128) // 512 if (i + kt) > 0 else 0
                        kcol = i * 128 + kt * 128
                        kbuf = kab[kv][0] if (i == 0 and kt == 0) else kab[kv][1 + (kcol - 128) // 512]
                        for jj in range(2):
                            j = 2 * kv + jj
                            col = (kt * 2 + jj) * 128
                            op("pe", lambda e, bank=bank, col=col, ps_=ps_, kcol=kcol, j=j, kv=kv, i=i:
                               e.matmul(bank[:, col:col + 128], lhsT=kaT3[ps_, kv, kcol:kcol + 128], rhs=qaT3[ps_, j, i * 128:(i + 1) * 128], start=True, stop=True),
                               reads=[kbuf, qab[j][q]], writes=[bankb])
                    op("act", lambda e, bank=bank, par=par, Pv=Pv: e.activation(out=Pv[par], in_=bank[:], func=AF.Exp, bias=negsmax, scale=0.125),
                       reads=[bankb, smb], writes=[Pb[par]])
                    op("dve", lambda e, par=par, Pv=Pv: e.tensor_tensor(out=Pv[par], in0=Pv[par], in1=mask4, op=ALU.mult), reads=[Pb[par], mask4b], writes=[Pb[par]])
                yield
                ob_, obb_ = self.psum()
                for par in range(2):
                    for jj in range(2):
                        hl = 2 * jj + par
                        for kt in range(2):
                            col = (kt * 2 + jj) * 128
                            op("pe", lambda e, ob_=ob_, hl=hl, par=par, col=col, kt=kt, kv=kv, i=i, Pv=Pv:
                               e.matmul(ob_[:, hl * 65:(hl + 1) * 65], lhsT=Pv[par][:, col:col + 128], rhs=vaug4[:, i + kt, kv, :], start=(kt == 0), stop=(kt == 1)),
                               reads=[Pb[par], vab[i + kt]], writes=[obb_])
                ii = slot
                ob3 = ob_[:, 0:260].rearrange("p (h e) -> p h e", e=65)
                op("dve", lambda e, ob3=ob3, kv=kv, ii=ii: e.tensor_tensor(out=den[ii][:, kv * 4:(kv + 1) * 4], in0=ob3[:, :, 64], in1=sinkexp[:, kv * 4:(kv + 1) * 4], op=ALU.add),
                   reads=[obb_, smb], writes=[denb[ii]])
                op("dve", lambda e, kv=kv, ii=ii: e.reciprocal(out=den[ii][:, kv * 4:(kv + 1) * 4], in_=den[ii][:, kv * 4:(kv + 1) * 4]), reads=[denb[ii]], writes=[denb[ii]])
                op("dve", lambda e, ob3=ob3, kv=kv, ii=ii: e.tensor_tensor(out=oat[ii][:, kv * 256:(kv + 1) * 256].rearrange("p (h e) -> p h e", e=64), in0=ob3[:, :, 0:64],
                                                                    in1=den[ii][:, kv * 4:(kv + 1) * 4].unsqueeze(2).to_broadcast([128, 4, 64]), op=ALU.mult),
                   reads=[obb_, denb[ii]], writes=[oatb[ii]])
            yield
            ii = slot
            ptr, ptrb = self.psum()
            ptr_bf = ptr[:].bitcast(BF16)
            for j in range(4):
                op("pe", lambda e, ptr_bf=ptr_bf, j=j, ii=ii: e.transpose(out=ptr_bf[:, j * 128:(j + 1) * 128], in_=oat[ii][:, j * 128:(j + 1) * 128], identity=self.ident),
                   reads=[oatb[ii], cmb], writes=[ptrb])
            op("act", lambda e, ptr_bf=ptr_bf, i=i: e.activation(out=oaT3[:, :, i * 128:(i + 1) * 128], in_=ptr_bf[:, 0:512].rearrange("p (j t) -> p j t", t=128), func=AF.Copy),
               reads=[ptrb], writes=[oab[q]])

        run_interleaved((swa_block(i, i % BW) for i in range(NT)), BW)

        off[0] = base
        glrT, glrTb = al(T, BF16, "glrT")
        HS = []
        for sl in range(2):
            h_ = {}
            for nm, n_, dt_ in [("A", T, F32), ("B", T, F32), ("C", T, F32), ("esp0", 512, F32), ("esp1", 512, F32), ("qtT", T, BF16), ("ktT", T, BF16),
                                ("khT", T, BF16), ("khat", NT * 128, BF16), ("vb", NT * 256, BF16), ("At0", 128, BF16), ("At1", 128, BF16),
                                ("rs", 512, F32), ("cst", NT, F32), ("dn", NT, F32)]:
                h_[nm], h_[nm + "b"] = al(n_, dt_, "g%d%s" % (sl, nm))
            h_["ograw"] = self.wslot[sl][:, 0:4096].bitcast(F32)
            h_["ograwb"] = self.wbuf[sl]
            h_["sqo"] = self.wslot[5][:, sl * 1024:(sl + 1) * 1024]
            h_["sqob"] = self.wbuf[5]
            HS.append(h_)

        ssrc = d["st_in"] if first else d["st_out"]
        op("sp", lambda e: e.dma_start(out=self.Sst[:], in_=ssrc[l].rearrange("h c v -> c h v")), reads=[self.stb[l]], writes=self.Sb, dma=True)
        for hd in range(4):
            op("act", lambda e, hd=hd: e.activation(out=self.Sbf[:, hd, :], in_=self.Sst[:, hd, :], func=AF.Copy), reads=[self.Sb[hd]], writes=[self.Sbfb[hd]])

        for q in range(NQ):
            qs = slice(q * 512, (q + 1) * 512)
            pt, pb = self.psum()
            for k in range(8):
                op("pe", lambda e, pt=pt, k=k, qs=qs: e.matmul(pt[0:16, :], lhsT=wglr[:, k, 0:16], rhs=hT[:, k, qs], start=(k == 0), stop=(k == 7)), reads=[wglrb, hb[q]], writes=[pb])
            op("act", lambda e, pt=pt, qs=qs: e.activation(out=glrT[0:16, qs], in_=pt[0:16, :], func=AF.Copy), reads=[pb], writes=[glrTb])

        def gla_head(hd, sl):
            H = HS[sl]
            sp, spb, csum, csumb, Ei, Eib = H["A"], H["Ab"], H["B"], H["Bb"], H["C"], H["Cb"]
            Ee, Eeb, bpos, bposb = sp, spb, csum, csumb
            esp, espb = [H["esp0"], H["esp1"]], [H["esp0b"], H["esp1b"]]
            qtT, qtTb, ktT, ktTb, khT, khTb = H["qtT"], H["qtTb"], H["ktT"], H["ktTb"], H["khT"], H["khTb"]
            khat3 = H["khat"].rearrange("p (n c) -> p n c", c=128)
            khatb = H["khatb"]
            vb3 = H["vb"].rearrange("p (n c) -> p n c", c=256)
            vbb = H["vbb"]
            ograw3 = H["ograw"].rearrange("p (a t) -> p a t", t=T)
            ograwb = H["ograwb"]
            At, Atb = [H["At0"], H["At1"]], [H["At0b"], H["At1b"]]
            sqo3 = H["sqo"].rearrange("p (a t) -> p a t", t=512)
            sqob, rs, rsb, cst, cstb, dn, dnb = H["sqob"], H["rs"], H["rsb"], H["cst"], H["cstb"], H["dn"], H["dnb"]
            csum3 = csum.rearrange("p (n t) -> p n t", t=128)
            srcs = []
            for (c0, w, dc0) in [(C_QB + hd * 128, 128, 0), (C_KB + hd * 128, 128, 128), (C_VB + hd * 256, 256, 256)]:
                src = d["w_in"][l, :, c0:c0 + w].rearrange("(k p) c -> p k c", p=128)
                srcs.append((lambda t, dc0=dc0, w=w: t[:, 0:4096].rearrange("p (k c) -> p k c", c=512)[:, :, dc0:dc0 + w], src))
            th, whb = self.wload(srcs, 3 + sl)
            wh = th[:, 0:4096].rearrange("p (k c) -> p k c", c=512)
            for q in range(NQ):
                qs = slice(q * 512, (q + 1) * 512)
                pz, pzb = self.psum()
                op("pe", lambda e, pz=pz, qs=qs: e.matmul(pz[:], lhsT=self.wgate[0:16, hd * 128:(hd + 1) * 128], rhs=glrT[0:16, qs], start=True, stop=True),
                   reads=[self.wgateb, glrTb], writes=[pzb])
                op("act", lambda e, pz=pz, q=q: e.activation(out=esp[q % 2], in_=pz[:], func=AF.Exp, scale=-1.0, bias=sm[:, 20 + hd:21 + hd]), reads=[pzb, smb], writes=[espb[q % 2]])
                op("act", lambda e, q=q, qs=qs: e.activation(out=sp[:, qs], in_=esp[q % 2], func=AF.Ln, bias=1.0), reads=[espb[q % 2]], writes=[spb])
            yield
            for q in range(NQ):
                qs = slice(q * 512, (q + 1) * 512)
                init = 0.0 if q == 0 else csum[:, q * 512 - 1:q * 512]
                op("dve", lambda e, qs=qs, init=init: e.tensor_tensor_scan(out=csum[:, qs], data0=self.onesf[:, 0:512], data1=sp[:, qs], initial=init, op0=ALU.mult, op1=ALU.add),
                   reads=[spb, self.onesb, csumb], writes=[csumb])
            op("dve", lambda e: e.memset(cst[:, 0:1], 0.0), writes=[cstb])
            op("dve", lambda e: e.tensor_copy(out=cst[:, 1:NT], in_=csum[:, 127:T - 1:128]), reads=[csumb], writes=[cstb])
            op("dve", lambda e: e.tensor_tensor(out=csum3, in0=csum3, in1=cst[:, 0:NT].unsqueeze(2).to_broadcast([128, NT, 128]), op=ALU.subtract), reads=[csumb, cstb], writes=[bposb])
            op("act", lambda e: e.activation(out=Ee, in_=bpos, func=AF.Exp, scale=-1.0 / 16), reads=[bposb, spb], writes=[Eeb])
            op("act", lambda e: e.activation(out=Ei, in_=bpos, func=AF.Exp, scale=1.0 / 16), reads=[bposb], writes=[Eib])
            op("dve", lambda e: e.tensor_copy(out=dn[:, 0:NT], in_=Ee[:, 127:T:128]), reads=[Eeb], writes=[dnb])
            yield
            for q in range(NQ):
                qs = slice(q * 512, (q + 1) * 512)
                pq, pqb = self.psum()
                for k in range(8):
                    op("pe", lambda e, pq=pq, k=k, qs=qs: e.matmul(pq[:], lhsT=wh[:, k, 0:128], rhs=hT[:, k, qs], start=(k == 0), stop=(k == 7)), reads=[whb, hb[q]], writes=[pqb])
                op("dve", lambda e, pq=pq, qs=qs: e.scalar_tensor_tensor(out=qtT[:, qs], in0=pq[:], scalar=float(128 ** -0.5), in1=Ee[:, qs], op0=ALU.mult, op1=ALU.mult),
                   reads=[pqb, Eeb], writes=[qtTb])
                pk, pkb = self.psum()
                for k in range(8):
                    op("pe", lambda e, pk=pk, k=k, qs=qs: e.matmul(pk[:], lhsT=wh[:, k, 128:256], rhs=hT[:, k, qs], start=(k == 0), stop=(k == 7)), reads=[whb, hb[q]], writes=[pkb])
                op("dve", lambda e, pk=pk, qs=qs: e.tensor_tensor(out=ktT[:, qs], in0=pk[:], in1=Ei[:, qs], op=ALU.mult), reads=[pkb, Eib], writes=[ktTb])
            op("dve", lambda e: e.tensor_tensor(out=khT.rearrange("p (n t) -> p n t", t=128), in0=ktT.rearrange("p (n t) -> p n t", t=128),
                                                in1=dn[:, 0:NT].unsqueeze(2).to_broadcast([128, NT, 128]), op=ALU.mult), reads=[ktTb, dnb], writes=[khTb])
            yield
            for n in range(NT):
                pv, pvb = self.psum()
                for k in range(8):
                    op("pe", lambda e, pv=pv, k=k, n=n: e.matmul(pv[:, 0:256], lhsT=hT[:, k, n * 128:(n + 1) * 128], rhs=wh[:, k, 256:512], start=(k == 0), stop=(k == 7)),
                       reads=[whb, hb[n // 4]], writes=[pvb])
                op("act", lambda e, pv=pv, n=n: e.activation(out=vb3[:, n, :], in_=pv[:, 0:256], func=AF.Copy), reads=[pvb], writes=[vbb])
                if n % 4 == 3:
                    yield
            for n0 in range(0, NT, 4):
                ptr, ptrb = self.psum()
                ptr_bf = ptr[:].bitcast(BF16)
                for nn in range(4):
                    n = n0 + nn
                    op("pe", lambda e, ptr_bf=ptr_bf, nn=nn, n=n: e.transpose(out=ptr_bf[:, nn * 128:(nn + 1) * 128], in_=khT[:, n * 128:(n + 1) * 128], identity=self.ident),
                       reads=[khTb, cmb], writes=[ptrb])
                op("act", lambda e, ptr_bf=ptr_bf, n0=n0: e.activation(out=khat3[:, n0:n0 + 4, :], in_=ptr_bf[:, 0:512].rearrange("p (n c) -> p n c", c=128), func=AF.Copy),
                   reads=[ptrb], writes=[khatb])
            yield
            for n in range(NT):
                ns = slice(n * 128, (n + 1) * 128)
                ai = n % 2
                pA, pAb = self.psum()
                op("pe", lambda e, pA=pA, ns=ns: e.matmul(pA[:, 0:128], lhsT=ktT[:, ns], rhs=qtT[:, ns], start=True, stop=True), reads=[ktTb, qtTb], writes=[pAb])
                op("dve", lambda e, pA=pA, ai=ai: e.tensor_tensor(out=At[ai], in0=pA[:, 0:128], in1=self.mcur, op=ALU.mult), reads=[pAb, cmb], writes=[Atb[ai]])
                pU, pUb = self.psum()
                op("pe", lambda e, pU=pU, n=n: e.matmul(pU[:, 0:256], lhsT=khat3[:, n, :], rhs=vb3[:, n, :], start=True, stop=True), reads=[khatb, vbb], writes=[pUb])
                yield
                po, pob = self.psum()
                for vc in range(2):
                    op("pe", lambda e, po=po, vc=vc, n=n, ai=ai: e.matmul(po[:, vc * 128:(vc + 1) * 128], lhsT=vb3[:, n, vc * 128:(vc + 1) * 128], rhs=At[ai], start=True, stop=False),
                       reads=[vbb, Atb[ai]], writes=[pob])
                    op("pe", lambda e, po=po, vc=vc, ns=ns: e.matmul(po[:, vc * 128:(vc + 1) * 128], lhsT=self.Sbf[:, hd, vc * 128:(vc + 1) * 128], rhs=qtT[:, ns], start=False, stop=True),
                       reads=[self.Sbfb[hd], qtTb], writes=[pob])
                op("act", lambda e, po=po, ns=ns: e.activation(out=ograw3[:, :, ns], in_=po[:, 0:256].rearrange("p (a t) -> p a t", t=128), func=AF.Copy), reads=[pob], writes=[ograwb])
                op("dve", lambda e, pU=pU, n=n: e.scalar_tensor_tensor(out=self.Sst[:, hd, :], in0=self.Sst[:, hd, :], scalar=dn[:, n:n + 1], in1=pU[:, 0:256], op0=ALU.mult, op1=ALU.add),
                   reads=[pUb, dnb, self.Sb[hd]], writes=[self.Sb[hd]])
                op("act", lambda e: e.activation(out=self.Sbf[:, hd, :], in_=self.Sst[:, hd, :], func=AF.Copy), reads=[self.Sb[hd]], writes=[self.Sbfb[hd]])
                yield
            for q in range(NQ):
                qs = slice(q * 512, (q + 1) * 512)
                op("pool", lambda e, qs=qs: e.tensor_tensor(out=sqo3, in0=ograw3[:, :, qs], in1=ograw3[:, :, qs], op=ALU.mult), reads=[ograwb], writes=[sqob])
                yield
                pss, pssb = self.psum()
                for vc in range(2):
                    op("pe", lambda e, pss=pss, vc=vc: e.matmul(pss[:], lhsT=self.ones, rhs=sqo3[:, vc, :], start=(vc == 0), stop=(vc == 1)), reads=[sqob, cmb], writes=[pssb])
                op("act", lambda e, pss=pss: e.activation(out=rs, in_=pss[:], func=AF.Ln, scale=1.0 / 256, bias=EPS), reads=[pssb], writes=[rsb])
                op("act", lambda e: e.activation(out=rs, in_=rs, func=AF.Exp, scale=-0.5), reads=[rsb], writes=[rsb])
                for vc in range(2):
                    j = hd * 2 + vc
                    op("dve", lambda e, vc=vc, j=j, qs=qs: e.scalar_tensor_tensor(out=obT3[:, j, qs], in0=ograw3[:, vc, qs], scalar=pcol[:, 24 + j:25 + j], in1=rs, op0=ALU.mult, op1=ALU.mult),
                       reads=[ograwb, rsb, pcolb], writes=[obb[j][q]])
                yield

        run_interleaved((gla_head(hd, hd % 2) for hd in range(4)), 2)
        o3 = op("sp", lambda e: e.dma_start(out=d["st_out"][l].rearrange("h c v -> c h v"), in_=self.Sst[:]), reads=self.Sb, writes=[self.stb[l]], dma=True)
        self.outs.append(o3)

        off[0] = base
        sr, srb = al2(512, BF16, "sr")
        wpa, wpab = self.wrows("w_proj_a", l, 0, 4, D, 2)
        wpb0, wpb0b = self.wrows("w_proj_b", l, 0, 4, D, 3)
        wpb1, wpb1b = self.wrows("w_proj_b", l, 512, 4, D, 4)
        ci = 0
        for half in range(2):
            wr, wrb = self.wcols("w_in", l, C_RB + half * 512, 512, half)
            for jj in range(4):
                j = half * 4 + jj
                for q in range(NQ):
                    qs = slice(q * 512, (q + 1) * 512)
                    pr, prb = self.psum()
                    for k in range(8):
                        op("pe", lambda e, pr=pr, k=k, jj=jj, qs=qs, wr=wr: e.matmul(pr[:], lhsT=wr[:, k, jj * 128:(jj + 1) * 128], rhs=hT[:, k, qs], start=(k == 0), stop=(k == 7)),
                           reads=[wrb, hb[q]], writes=[prb])
                    c2 = ci % 2
                    ci += 1
                    op("act", lambda e, pr=pr, c2=c2: e.activation(out=sr[c2], in_=pr[:], func=AF.Silu), reads=[prb], writes=[srb[c2]])
                    op("pool", lambda e, j=j, qs=qs, c2=c2: e.tensor_tensor(out=obT3[:, j, qs], in0=obT3[:, j, qs], in1=sr[c2], op=ALU.mult), reads=[srb[c2], obb[j][q]], writes=[obb[j][q]])

        mT, _ = al(8 * T, BF16, "mT")
        mT3 = mT.rearrange("p (j t) -> p j t", t=T)
        mb = [[Buf("m%d_%d" % (j, q)) for q in range(NQ)] for j in range(8)]
        for row in mb:
            for bb in row:
                self.arena_hist.append((off[0] - 8 * T, off[0], bb))
        sga, sgab = al2(512, F32, "sga")
        sgb_, sgbb = al2(512, F32, "sgb")
        m1, m1b = al2(512, F32, "m1")
        m2, m2b = al2(512, F32, "m2")
        wpb = [(wpb0, wpb0b), (wpb1, wpb1b)]
        gslots = [(5, 0), (1, 5)]
        ci = 0
        for half in range(2):
            wga, wgab = self.wcols("w_in", l, C_GA + half * 512, 512, gslots[half][0])
            wgb, wgbb = self.wcols("w_in", l, C_GB + half * 512, 512, gslots[half][1])
            for dcc in range(4):
                dc = half * 4 + dcc
                ds_ = slice(dc * 128, (dc + 1) * 128)
                for q in range(NQ):
                    qs = slice(q * 512, (q + 1) * 512)
                    c2 = ci % 2
                    ci += 1
                    pya, pyab = self.psum()
                    for j in range(4):
                        op("pe", lambda e, pya=pya, j=j, ds_=ds_, qs=qs: e.matmul(pya[:], lhsT=wpa[:, j, ds_], rhs=oaT3[:, j, qs], start=(j == 0), stop=(j == 3)), reads=[wpab, oab[q]], writes=[pyab])
                    pyb, pybb = self.psum()
                    for j in range(8):
                        w_, wb_ = wpb[j // 4]
                        op("pe", lambda e, pyb=pyb, j=j, ds_=ds_, qs=qs, w_=w_: e.matmul(pyb[:], lhsT=w_[:, j % 4, ds_], rhs=obT3[:, j, qs], start=(j == 0), stop=(j == 7)), reads=[wb_, obb[j][q]], writes=[pybb])
                    pga, pgab = self.psum()
                    for k in range(8):
                        op("pe", lambda e, pga=pga, k=k, dcc=dcc, qs=qs, wga=wga: e.matmul(pga[:], lhsT=wga[:, k, dcc * 128:(dcc + 1) * 128], rhs=hT[:, k, qs], start=(k == 0), stop=(k == 7)), reads=[wgab, hb[q]], writes=[pgab])
                    pgb, pgbb = self.psum()
                    for k in range(8):
                        op("pe", lambda e, pgb=pgb, k=k, dcc=dcc, qs=qs, wgb=wgb: e.matmul(pgb[:], lhsT=wgb[:, k, dcc * 128:(dcc + 1) * 128], rhs=hT[:, k, qs], start=(k == 0), stop=(k == 7)), reads=[wgbb, hb[q]], writes=[pgbb])
                    op("act", lambda e, pga=pga, c2=c2: e.activation(out=sga[c2], in_=pga[:], func=AF.Sigmoid), reads=[pgab], writes=[sgab[c2]])
                    op("act", lambda e, pgb=pgb, c2=c2: e.activation(out=sgb_[c2], in_=pgb[:], func=AF.Sigmoid), reads=[pgbb], writes=[sgbb[c2]])
                    op("dve", lambda e, pya=pya, c2=c2: e.tensor_tensor(out=m1[c2], in0=pya[:], in1=sga[c2], op=ALU.mult), reads=[pyab, sgab[c2]], writes=[m1b[c2]])
                    op("dve", lambda e, pyb=pyb, c2=c2: e.tensor_tensor(out=m2[c2], in0=pyb[:], in1=sgb_[c2], op=ALU.mult), reads=[pybb, sgbb[c2]], writes=[m2b[c2]])
                    op("pool", lambda e, dc=dc, qs=qs, c2=c2: e.tensor_tensor(out=mT3[:, dc, qs], in0=m1[c2], in1=m2[c2], op=ALU.add), reads=[m1b[c2], m2b[c2]], writes=[mb[dc][q]])
        wo = [self.wrows("w_out", l, 0, 4, D, 4), self.wrows("w_out", l, 512, 4, D, 5)]
        if self.stop_after is None:
            self.pre2 = self.ffn_prefetch(l, "ffn2")
        for dc in range(8):
            ds_ = slice(dc * 128, (dc + 1) * 128)
            for q in range(NQ):
                qs = slice(q * 512, (q + 1) * 512)
                po, pob = self.psum()
                for j in range(8):
                    w_, wb_ = wo[j // 4]
                    op("pe", lambda e, po=po, j=j, ds_=ds_, qs=qs, w_=w_: e.matmul(po[:], lhsT=w_[:, j % 4, ds_], rhs=mT3[:, j, qs], start=(j == 0), stop=(j == 7)), reads=[wb_, mb[j][q]], writes=[pob])
                op("dve", lambda e, po=po, dc=dc, qs=qs: e.tensor_tensor(out=xT[:, dc, qs], in0=po[:], in1=xT[:, dc, qs], op=ALU.add), reads=[pob, self.xb[dc][q]], writes=[self.xb[dc][q]])
        if self.dbg:
            for j in range(4):
                self.outs.append(op("sp", lambda e, j=j: e.dma_start(out=d["dbg_oa"][j], in_=oaT3[:, j, :]), reads=oab, writes=[Buf("dbgoa")], dma=True))
            for j in range(8):
                self.outs.append(op("sp", lambda e, j=j: e.dma_start(out=d["dbg_ob"][j], in_=obT3[:, j, :]), reads=obb[j], writes=[Buf("dbgob")], dma=True))


def _consts_host():
    cm = np.zeros((128, 6, 128), np.float32)
    cm[:, 0] = np.eye(128)
    cm[:, 1] = 1.0
    for h in range(2):
        cm[h * 64:(h + 1) * 64, 2, h * 64:(h + 1) * 64] = 1.0
    for m in range(128):
        if m % 64 < 32:
            cm[m + 32, 3, m] = -1.0
        else:
            cm[m - 32, 3, m] = 1.0
    kk = np.arange(128)[:, None]
    qq = np.arange(128)[None, :]
    cm[:, 4] = (kk <= qq)
    cm[:, 5] = (kk > qq)
    return cm.reshape(128, 6 * 128)


def _rope_host(pos0, T):
    inv = (10000.0 ** (-np.arange(0, 64, 2, dtype=np.float32) / 64)).astype(np.float32)
    pos = np.arange(pos0, pos0 + T, dtype=np.float32)
    ang = pos[None, :] * inv[:, None]
    c = np.cos(ang).astype(np.float32)
    s = np.sin(ang).astype(np.float32)
    c128 = np.tile(c, (4, 1))
    s128 = np.tile(s, (4, 1))
    return np.stack([c128, s128], 0)


def _pcol_host(inp, l):
    pc = np.zeros((128, NPCOL), np.float32)
    pc[:, 0:8] = inp["ffn1_norm"][l].reshape(8, 128).T
    pc[:, 8:16] = inp["mix_norm"][l].reshape(8, 128).T
    pc[:, 16:24] = inp["ffn2_norm"][l].reshape(8, 128).T
    pc[:, 24:32] = inp["gla_out_norm"][l].reshape(8, 128).T
    pc[:, 32] = np.tile(inp["swa_q_norm"][l], 2)
    pc[:, 33] = np.tile(inp["swa_k_norm"][l], 2)
    pc[:, 34:38] = inp["gla_gate_bias"][l].reshape(4, 128).T
    pc[:, 38:46] = inp["swa_sinks"][l][None, :]
    pr = np.concatenate([inp["swa_q_norm"][l], inp["swa_k_norm"][l]])[None, :].astype(np.float32)
    return pc, pr


WNAMES = ["ffn1_w_gate", "ffn1_w_up", "ffn1_w_down", "ffn2_w_gate", "ffn2_w_up", "ffn2_w_down",
          "w_in", "gla_w_gate", "w_proj_a", "w_proj_b", "w_out"]

T_SEG = 1024
NSEG = SEQ // T_SEG
N_ACTIVE = 2


def _build():
    steps = [(s, l) for s in range(NSEG) for l in range(DEPTH)]
    P = Prog(T_SEG, NSEG, DEPTH, steps)
    return P.build()


def kernel(**inputs):
    inp = {k: np.ascontiguousarray(np.asarray(v, dtype=np.float32)) for k, v in inputs.items()}
    nc = _build()
    pcs, prs = zip(*[_pcol_host(inp, l) for l in range(DEPTH)])
    pcol = np.stack(pcs)
    prow = np.stack(prs)
    cm = _consts_host()
    rope = np.stack([_rope_host(s * T_SEG, T_SEG) for s in range(NSEG)])
    in_maps = []
    for b in range(N_ACTIVE):
        xin = np.ascontiguousarray(inp["x"][b].reshape(NSEG, T_SEG, D).transpose(0, 2, 1))
        m = {"xin": xin, "rope": rope,
             "st_in": np.zeros((DEPTH, 4, 128, 256), np.float32),
             "kh_in": np.zeros((DEPTH, 2, 128, 128), np.float32),
             "vh_in": np.zeros((DEPTH, 128, 130), np.float32),
             "cmat": cm, "pcol": pcol, "prow": prow}
        for n in WNAMES:
            m[n] = inp[n]
        in_maps.append(m)
    res = run_bass_kernel_spmd(nc, in_maps, core_ids=list(range(N_ACTIVE)))
    out = np.empty((B_, SEQ, D), np.float32)
    for b in range(N_ACTIVE):
        xo = np.asarray(res.results[b]["xout"])
        out[b] = xo.transpose(0, 2, 1).reshape(SEQ, D)
    return out
```

```python
import contextlib
import numpy as np
import concourse.bass as bass
import concourse.mybir as mybir
from concourse.bass_utils import run_bass_kernel_spmd

F32 = mybir.dt.float32
BF16 = mybir.dt.bfloat16
AF = mybir.ActivationFunctionType
ALU = mybir.AluOpType

D = 1024
DFF = 2816
B_ = 2
SEQ = 8192
DEPTH = 4
EPS = 1e-6
INC = 5904
C_QA, C_KA, C_VA, C_QB, C_KB, C_VB, C_RB, C_GLR, C_GA, C_GB = 0, 512, 640, 768, 1280, 1792, 2816, 3840, 3856, 4880
NPCOL = 48
ENGS = ("pe", "act", "dve", "pool", "sp")


class Buf:
    __slots__ = ("name", "writer", "readers", "excl")

    def __init__(self, name, excl=False, after=()):
        self.name = name
        self.writer = None
        self.readers = []
        self.excl = excl
        if after:
            newest = {}
            dmas = {}
            for o in after:
                for a in ([o.writer] if o.writer is not None else []) + o.readers:
                    if a.dma:
                        dmas[id(a)] = a
                    elif a.eng not in newest or newest[a.eng].seq < a.seq:
                        newest[a.eng] = a
            self.readers = list(newest.values()) + list(dmas.values())


class Op:
    __slots__ = ("eng", "fn", "waits", "signal", "dma", "semkey", "semval", "seq")
    _n = 0

    def __init__(self, eng, fn, dma):
        Op._n += 1
        self.seq = Op._n
        self.eng = eng
        self.fn = fn
        self.dma = dma
        self.waits = []
        self.signal = False
        self.semkey = None
        self.semval = None


class Sched:
    def __init__(self, nc):
        self.nc = nc
        self.ops = {e: [] for e in ENGS}
        self.all_ops = []

    def op(self, eng, fn, reads=(), writes=(), dma=False, semkey=None):
        o = Op(eng, fn, dma)
        deps = []
        for b in reads:
            if b.writer is not None:
                deps.append(b.writer)
            if b.excl:
                deps.extend(b.readers)
        for b in writes:
            if b.writer is not None:
                deps.append(b.writer)
            deps.extend(b.readers)
        seen = set()
        for d in deps:
            if d is o or id(d) in seen:
                continue
            seen.add(id(d))
            if d.eng == "pe" and eng == "pe" and not d.dma and not dma:
                continue
            o.waits.append(d)
        for b in reads:
            if b.excl:
                b.writer = o
                b.readers = []
            else:
                if not dma:
                    b.readers = [r for r in b.readers if r.dma or r.eng != eng]
                b.readers.append(o)
        for b in writes:
            b.writer = o
            b.readers = []
        if dma:
            o.semkey = semkey if semkey is not None else writes[0]
        self.ops[eng].append(o)
        self.all_ops.append(o)
        return o

    def emit(self, final_wait_ops=()):
        nc = self.nc
        for o in self.all_ops:
            for d in o.waits:
                d.signal = True
        for o in final_wait_ops:
            o.signal = True
        for o in self.all_ops:
            if o.dma:
                o.signal = True
        with contextlib.ExitStack() as stack:
            eng_sem = {e: stack.enter_context(nc.semaphore("s_" + e)) for e in ENGS}
            dma_sem = {}
            cnt = {e: 0 for e in ENGS}
            dcount = {}
            for o in self.all_ops:
                if not o.signal:
                    continue
                if o.dma:
                    key = o.semkey
                    if key not in dma_sem:
                        dma_sem[key] = stack.enter_context(nc.semaphore("d_%d" % len(dma_sem)))
                        dcount[key] = 0
                    dcount[key] += 16
                    o.semval = (dma_sem[key], dcount[key])
                else:
                    cnt[o.eng] += 1
                    o.semval = (eng_sem[o.eng], cnt[o.eng])
            self.stats = dict(cnt=cnt, n_dma_sems=len(dma_sem), n_ops={e: len(self.ops[e]) for e in ENGS})
            block = stack.enter_context(nc.Block())
            engobj = {"pe": "tensor", "act": "scalar", "dve": "vector", "pool": "gpsimd", "sp": "sync"}

            def make(ename):
                ops = self.ops[ename]

                def body(eng):
                    known = {}
                    for o in ops:
                        need = {}
                        for d in o.waits:
                            s, v = d.semval
                            if need.get(s, 0) < v:
                                need[s] = v
                        for s, v in need.items():
                            if known.get(s, 0) >= v:
                                continue
                            eng.wait_ge(s, v)
                            known[s] = v
                        ins = o.fn(eng)
                        if o.signal:
                            s, v = o.semval
                            ins.then_inc(s, 16 if o.dma else 1)
                    if ename == "sp":
                        for o in final_wait_ops:
                            s, v = o.semval
                            eng.wait_ge(s, v)
                return body

            for e in ENGS:
                getattr(block, engobj[e])(make(e))


def run_interleaved(gens, width):
    it = iter(gens)
    active = []
    more = True
    while True:
        while more and len(active) < width:
            try:
                active.append(next(it))
            except StopIteration:
                more = False
        if not active:
            break
        for g in list(active):
            try:
                next(g)
            except StopIteration:
                active.remove(g)


class Prog:
    def __init__(self, T, nseg, nl, steps, stop_after=None, dbg=False):
        self.dbg = dbg
        self.T, self.NSEG, self.NL, self.steps = T, nseg, nl, steps
        self.NQ, self.NT = T // 512, T // 128
        self.stop_after = stop_after
        nc = self.nc = bass.Bass("TRN2", target_bir_lowering=False)
        self.S = Sched(nc)
        self.stack = contextlib.ExitStack()
        self.outs = []
        self.ps_rr = 0

    def din(self, name, shape, dt=F32):
        return self.nc.dram_tensor(name, list(shape), dt, kind="ExternalInput").ap()

    def dout(self, name, shape, dt=F32):
        return self.nc.dram_tensor(name, list(shape), dt, kind="ExternalOutput").ap()

    def sb(self, name, shape, dt):
        t = self.stack.enter_context(self.nc.sbuf_tensor("sb_" + name, list(shape), dt))
        return t

    def op(self, *a, **k):
        return self.S.op(*a, **k)

    def psum(self):
        i = self.ps_rr
        self.ps_rr = (i + 1) % 8
        return self.ps_t[i], self.ps_b[i]

    def build(self):
        with self.stack:
            self._alloc()
            self._consts()
            for (sg, l) in self.steps:
                self._seglayer(sg, l)
            self.S.emit(final_wait_ops=self.outs)
        return self.nc

    def _alloc(self):
        T, NL, NSEG = self.T, self.NL, self.NSEG
        d = self.dram = {}
        d["xin"] = self.din("xin", [NSEG, D, T])
        d["xout"] = self.dout("xout", [NSEG, D, T])
        d["rope"] = self.din("rope", [NSEG, 2, 128, T])
        d["st_in"] = self.din("st_in", [NL, 4, 128, 256])
        d["st_out"] = self.dout("st_out", [NL, 4, 128, 256])
        d["kh_in"] = self.din("kh_in", [NL, 2, 128, 128])
        d["kh_out"] = self.dout("kh_out", [NL, 2, 128, 128])
        d["vh_in"] = self.din("vh_in", [NL, 128, 130])
        d["vh_out"] = self.dout("vh_out", [NL, 128, 130])
        d["cmat"] = self.din("cmat", [128, 6 * 128])
        d["pcol"] = self.din("pcol", [NL, 128, NPCOL])
        d["prow"] = self.din("prow", [NL, 1, 128])
        for n, shp in [("ffn1_w_gate", [D, DFF]), ("ffn1_w_up", [D, DFF]), ("ffn1_w_down", [DFF, D]),
                       ("ffn2_w_gate", [D, DFF]), ("ffn2_w_up", [D, DFF]), ("ffn2_w_down", [DFF, D]),
                       ("w_in", [D, INC]), ("gla_w_gate", [16, 512]), ("w_proj_a", [512, D]),
                       ("w_proj_b", [D, D]), ("w_out", [D, D])]:
            d[n] = self.din(n, [NL] + shp)

        sb = self.sb
        self.xT = sb("xT", [128, 8, T], F32)
        self.hT = sb("hT", [128, 8, T], BF16)
        self.xb = [[Buf("x%d_%d" % (k, q)) for q in range(self.NQ)] for k in range(8)]
        self.hb = [Buf("h%d" % q) for q in range(self.NQ)]
        self.NSLOT = 6
        self.wslot = [sb("w%d" % i, [128, 4096], BF16) for i in range(self.NSLOT)]
        self.wbuf = [Buf("w%d" % i) for i in range(self.NSLOT)]
        self.w_rr = 0
        self.AR = 45056
        self.arena = sb("arena", [128, self.AR], BF16)
        self.arena_hist = []
        self.ps_t = [self.stack.enter_context(self.nc.psum_tensor("ps%d" % i, [128, 512], F32)) for i in range(8)]
        self.ps_b = [Buf("ps%d" % i, excl=True) for i in range(8)]
        self.cm = sb("cm", [128, 6 * 128], BF16)
        self.cmb = Buf("cm")
        self.onesf = sb("onesf", [128, 512], F32)
        self.onesb = Buf("onesf")
        self.pcol = sb("pcol", [128, NPCOL], F32)
        self.pcolb = Buf("pcol")
        self.prow = sb("prow", [1, 128], F32)
        self.prowb = Buf("prow")
        self.small = sb("small", [128, 64], F32)
        self.smallb = Buf("small")
        self.wgate = sb("wgate", [16, 512], BF16)
        self.wgateb = Buf("wgate")
        self.rope = sb("ropet", [128, 2, T], F32)
        self.ropeb = Buf("rope")
        self.Sst = sb("Sst", [128, 4, 256], F32)
        self.Sbf = sb("Sbf", [128, 4, 256], BF16)
        self.Sb = [Buf("S%d" % h) for h in range(4)]
        self.Sbfb = [Buf("Sbf%d" % h) for h in range(4)]
        self.tiny = sb("tiny", [1, 256], F32)
        self.tinyb = Buf("tiny")
        self.stb = [Buf("st%d" % i) for i in range(NL)]
        self.khb = [Buf("kh%d" % i) for i in range(NL)]
        self.vhb = [Buf("vh%d" % i) for i in range(NL)]
        if self.dbg:
            d["dbg_oa"] = self.dout("dbg_oa", [4, 128, T], BF16)
            d["dbg_ob"] = self.dout("dbg_ob", [8, 128, T], BF16)
        self.cur_x = None

    def acompact(self):
        if len(self.arena_hist) > 1:
            f = Buf("fence", after=[hb for (_, _, hb) in self.arena_hist])
            self.arena_hist = [(0, self.AR, f)]

    def aview(self, off, n, dt, name):
        nb = 2 * n if dt == F32 else n
        assert off + nb <= self.AR, (name, off, nb, self.AR)
        if dt == F32:
            assert off % 2 == 0
            v = self.arena[:, off:off + nb].bitcast(F32)
        else:
            v = self.arena[:, off:off + nb]
        b = Buf(name, after=[hb for (s0, e0, hb) in self.arena_hist if s0 < off + nb and off < e0])
        self.arena_hist.append((off, off + nb, b))
        return v, b

    def _consts(self):
        cm, d = self.cm, self.dram
        self.op("pool", lambda e: e.dma_start(out=cm[:], in_=d["cmat"]), writes=[self.cmb], dma=True)
        self.op("pool", lambda e: e.memset(self.onesf[:], 1.0), writes=[self.onesb])
        self.ident = cm[:, 0:128]
        self.ones = cm[:, 128:256]
        self.blk = cm[:, 256:384]
        self.rot = cm[:, 384:512]
        self.mcur = cm[:, 512:640]
        self.mprev = cm[:, 640:768]

    def wload(self, srcs, slot):
        t, b = self.wslot[slot], self.wbuf[slot]
        for dv, src in srcs:
            self.op("pool", lambda e, dv=dv, src=src, t=t: e.dma_start(out=dv(t), in_=src), writes=[b], dma=True)
        return t, b

    def wcols(self, wname, l, c0, w, slot):
        src = self.dram[wname][l, :, c0:c0 + w].rearrange("(k p) c -> p k c", p=128)
        t, b = self.wload([(lambda t, w=w: t[:, 0:8 * w].rearrange("p (k c) -> p k c", c=w), src)], slot)
        return t[:, 0:8 * w].rearrange("p (k c) -> p k c", c=w), b

    def wrows(self, wname, l, r0, nj, ncol, slot):
        src = self.dram[wname][l, r0:r0 + nj * 128, 0:ncol].rearrange("(j p) c -> p j c", p=128)
        t, b = self.wload([(lambda t: t[:, 0:nj * ncol].rearrange("p (j c) -> p j c", c=ncol), src)], slot)
        return t[:, 0:nj * ncol].rearrange("p (j c) -> p j c", c=ncol), b

    def rmsnorm_h(self, gcol):
        T, NQ = self.T, self.NQ
        self.acompact()
        sq, sqb = zip(*[self.aview(i * 512, 512, BF16, "sq%d" % i) for i in range(4)])
        rstd, rstdb = self.aview(2048, 512, F32, "rstd")
        n = 0
        for q in range(NQ):
            qs = slice(q * 512, (q + 1) * 512)
            pt, pb = self.psum()
            for k in range(8):
                s, sb_ = sq[n % 4], sqb[n % 4]
                n += 1
                self.op("pool" if n % 3 == 0 else "dve", lambda e, s=s, k=k, qs=qs: e.tensor_tensor(out=s, in0=self.xT[:, k, qs], in1=self.xT[:, k, qs], op=ALU.mult),
                        reads=[self.xb[k][q]], writes=[sb_])
                self.op("pe", lambda e, s=s, k=k, pt=pt: e.matmul(pt[:], lhsT=self.ones, rhs=s, start=(k == 0), stop=(k == 7)),
                        reads=[sb_, self.cmb], writes=[pb])
            self.op("act", lambda e, pt=pt: e.activation(out=rstd, in_=pt[:], func=AF.Ln, scale=1.0 / D, bias=EPS), reads=[pb], writes=[rstdb])
            self.op("act", lambda e: e.activation(out=rstd, in_=rstd, func=AF.Exp, scale=-0.5), reads=[rstdb], writes=[rstdb])
            for k in range(8):
                self.op("dve", lambda e, k=k, qs=qs: e.scalar_tensor_tensor(out=self.hT[:, k, qs], in0=self.xT[:, k, qs], scalar=self.pcol[:, gcol + k:gcol + k + 1],
                                                                          in1=rstd, op0=ALU.mult, op1=ALU.mult),
                        reads=[self.xb[k][q], rstdb, self.pcolb], writes=[self.hb[q]])

    def ffn_prefetch(self, l, which):
        wg, wgb = self.wcols(which + "_w_gate", l, 0, 512, 0)
        wu, wub = self.wcols(which + "_w_up", l, 0, 512, 2)
        return (wg, wgb, wu, wub)

    def ffn(self, l, which, gcol, pre=None, post=None):
        T, NQ = self.T, self.NQ
        if pre is None:
            pre = self.ffn_prefetch(l, which)
        self.rmsnorm_h(gcol)
        wg_n, wu_n, wd_n = which + "_w_gate", which + "_w_up", which + "_w_down"
        groups = [(g * 512, 512) for g in range(5)] + [(2560, 256)]
        aT = [self.arena[:, i * 4 * T:(i + 1) * 4 * T].rearrange("p (j t) -> p j t", t=T) for i in range(2)]
        aTb = [[[self.aview(i * 4 * T + j * T + q * 512, 512, BF16, "a%d_%d_%d" % (i, j, q))[1] for q in range(NQ)] for j in range(4)] for i in range(2)]
        sg, sgb = zip(*[self.aview(8 * T + i * 1024, 512, F32, "sg%d" % i) for i in range(2)])
        nsg = 0

        def down(gi, f0, fw, wd, wdb):
            nj = fw // 128
            a, ab = aT[gi % 2], aTb[gi % 2]
            for dc in range(8):
                for q in range(NQ):
                    qs = slice(q * 512, (q + 1) * 512)
                    pt, pb = self.psum()
                    for j in range(nj):
                        self.op("pe", lambda e, pt=pt, j=j, dc=dc, qs=qs, a=a, wd=wd: e.matmul(pt[:], lhsT=wd[:, j, dc * 128:(dc + 1) * 128], rhs=a[:, j, qs],
                                                                                               start=(j == 0), stop=(j == nj - 1)),
                                reads=[wdb, ab[j][q]], writes=[pb])
                    self.op("dve", lambda e, pt=pt, dc=dc, qs=qs: e.scalar_tensor_tensor(out=self.xT[:, dc, qs], in0=pt[:], scalar=0.5, in1=self.xT[:, dc, qs],
                                                                                         op0=ALU.mult, op1=ALU.add),
                            reads=[pb, self.xb[dc][q]], writes=[self.xb[dc][q]])

        pending = None
        for gi, (f0, fw) in enumerate(groups):
            nj = fw // 128
            if gi == 0:
                wg, wgb, wu, wub = pre
            else:
                wg, wgb = self.wcols(wg_n, l, f0, fw, gi % 2)
                wu, wub = self.wcols(wu_n, l, f0, fw, 2 + gi % 2)
            wd, wdb = self.wrows(wd_n, l, f0, nj, D, 4 + gi % 2)
            a, ab = aT[gi % 2], aTb[gi % 2]
            for j in range(nj):
                for q in range(NQ):
                    qs = slice(q * 512, (q + 1) * 512)
                    pg, pgb = self.psum()
                    pu, pub = self.psum()
                    for k in range(8):
                        self.op("pe", lambda e, pg=pg, k=k, j=j, qs=qs, wg=wg: e.matmul(pg[:], lhsT=wg[:, k, j * 128:(j + 1) * 128], rhs=self.hT[:, k, qs],
                                                                                        start=(k == 0), stop=(k == 7)),
                                reads=[wgb, self.hb[q]], writes=[pgb])
                    for k in range(8):
                        self.op("pe", lambda e, pu=pu, k=k, j=j, qs=qs, wu=wu: e.matmul(pu[:], lhsT=wu[:, k, j * 128:(j + 1) * 128], rhs=self.hT[:, k, qs],
                                                                                        start=(k == 0), stop=(k == 7)),
                                reads=[wub, self.hb[q]], writes=[pub])
                    s_, sb_ = sg[nsg % 2], sgb[nsg % 2]
                    nsg += 1
                    self.op("act", lambda e, pg=pg, s_=s_: e.activation(out=s_, in_=pg[:], func=AF.Silu), reads=[pgb], writes=[sb_])
                    self.op("dve", lambda e, pu=pu, s_=s_, a=a, j=j, qs=qs: e.tensor_tensor(out=a[:, j, qs], in0=pu[:], in1=s_, op=ALU.mult),
                            reads=[pub, sb_], writes=[ab[j][q]])
            if pending is not None:
                down(*pending)
            pending = (gi, f0, fw, wd, wdb)
        if post is not None:
            post()
        down(*pending)

    def _seglayer(self, sg, l):
        T, NQ, NT, d = self.T, self.NQ, self.NT, self.dram
        if self.cur_x != sg:
            for k in range(8):
                self.op("sp", lambda e, k=k: e.dma_start(out=self.xT[:, k, :], in_=d["xin"][sg, k * 128:(k + 1) * 128, :]),
                        writes=[self.xb[k][q] for q in range(NQ)], dma=True)
            self.op("sp", lambda e: e.dma_start(out=self.rope[:], in_=d["rope"][sg].rearrange("c p t -> p c t")), writes=[self.ropeb], dma=True)
            self.cur_x = sg
        self.op("sp", lambda e: e.dma_start(out=self.pcol[:], in_=d["pcol"][l]), writes=[self.pcolb], dma=True)
        self.op("sp", lambda e: e.dma_start(out=self.prow[:], in_=d["prow"][l]), writes=[self.prowb], dma=True)
        self.op("pool", lambda e: e.dma_start(out=self.wgate[:], in_=d["gla_w_gate"][l]), writes=[self.wgateb], dma=True)

        pre1 = getattr(self, "pre_next", None)
        self.pre_next = None
        self.ffn(l, "ffn1", 0, pre=pre1)
        if self.stop_after != "ffn1":
            self.pre2 = None
            self.mixer(sg, l)
            if self.stop_after != "mixer":
                idx = self.steps.index((sg, l))
                nxt = self.steps[idx + 1] if idx + 1 < len(self.steps) else None

                def post():
                    if nxt is not None:
                        self.pre_next = self.ffn_prefetch(nxt[1], "ffn1")
                self.ffn(l, "ffn2", 16, pre=self.pre2, post=post)
        last_layer_for_seg = not any((s2 == sg and l2 > l) for (s2, l2) in self.steps)
        if last_layer_for_seg:
            for k in range(8):
                o = self.op("sp", lambda e, k=k: e.dma_start(out=d["xout"][sg, k * 128:(k + 1) * 128, :], in_=self.xT[:, k, :]),
                            reads=[self.xb[k][q] for q in range(NQ)], writes=[Buf("xo")], dma=True, semkey="xo%d" % k)
                self.outs.append(o)
            self.cur_x = None

    def mixer(self, sg, l):
        T, NQ, NT, d = self.T, self.NQ, self.NT, self.dram
        op, pcol, pcolb, hT, hb, xT = self.op, self.pcol, self.pcolb, self.hT, self.hb, self.xT
        cmb, sm, smb = self.cmb, self.small, self.smallb
        first = not any((l2 == l) for (s2, l2) in self.steps[:self.steps.index((sg, l))])
        wqa, wqab = self.wcols("w_in", l, C_QA, 512, 0)
        srcs = []
        for kv in range(2):
            src = d["w_in"][l, :, C_KA + kv * 64:C_KA + (kv + 1) * 64].rearrange("(k p) c -> p k c", p=128)
            for dup in range(2):
                c0 = kv * 128 + dup * 64
                srcs.append((lambda t, c0=c0: t[:, 0:2048].rearrange("p (k c) -> p k c", c=256)[:, :, c0:c0 + 64], src))
        srcs.append((lambda t: t[:, 2048:3072].rearrange("p (k c) -> p k c", c=128),
                     d["w_in"][l, :, C_VA:C_VA + 128].rearrange("(k p) c -> p k c", p=128)))
        tkv, wkvb = self.wload(srcs, 1)
        wka = tkv[:, 0:2048].rearrange("p (k c) -> p k c", c=256)
        wva = tkv[:, 2048:3072].rearrange("p (k c) -> p k c", c=128)
        wglr, wglrb = self.wcols("w_in", l, C_GLR, 16, 2)
        self.rmsnorm_h(8)

        off = [0]

        def al(n, dt, name):
            if dt == F32 and off[0] % 2:
                off[0] += 1
            v, b = self.aview(off[0], n, dt, name)
            off[0] += 2 * n if dt == F32 else n
            return v, b

        def al2(n, dt, name):
            r = [al(n, dt, "%s%d" % (name, i)) for i in range(2)]
            return [x[0] for x in r], [x[1] for x in r]

        oaT, _ = al(4 * T, BF16, "oaT")
        oaT3 = oaT.rearrange("p (j t) -> p j t", t=T)
        oab = [Buf("oa%d" % q) for q in range(NQ)]
        obT, _ = al(8 * T, BF16, "obT")
        obT3 = obT.rearrange("p (j t) -> p j t", t=T)
        obb = [[Buf("ob%d_%d" % (j, q)) for q in range(NQ)] for j in range(8)]
        base = off[0]
        for bb in oab:
            self.arena_hist.append((0, 4 * T, bb))
        for row in obb:
            for bb in row:
                self.arena_hist.append((4 * T, 12 * T, bb))

        tiny, tinyb = self.tiny, self.tinyb
        op("dve", lambda e: e.tensor_tensor(out=tiny[0:1, 0:128], in0=self.prow[0:1, :], in1=self.prow[0:1, :], op=ALU.mult), reads=[self.prowb], writes=[tinyb])
        op("dve", lambda e: e.reduce_max(out=tiny[0:1, 128:130], in_=tiny[0:1, 0:128].rearrange("p (a b) -> p a b", b=64), axis=mybir.AxisListType.X),
           reads=[tinyb], writes=[tinyb])
        op("dve", lambda e: e.tensor_tensor(out=tiny[0:1, 130:131], in0=tiny[0:1, 128:129], in1=tiny[0:1, 129:130], op=ALU.mult), reads=[tinyb], writes=[tinyb])
        pt, pb = self.psum()
        op("pe", lambda e, pt=pt: e.matmul(pt[:, 0:1], lhsT=self.onesf[0:1, 0:128], rhs=tiny[0:1, 130:131], start=True, stop=True), reads=[tinyb, self.onesb], writes=[pb])
        op("act", lambda e, pt=pt: e.activation(out=sm[:, 1:2], in_=pt[:, 0:1], func=AF.Ln), reads=[pb], writes=[smb])
        op("act", lambda e: e.activation(out=sm[:, 2:3], in_=sm[:, 1:2], func=AF.Exp, scale=0.5), reads=[smb], writes=[smb])
        op("dve", lambda e: e.tensor_scalar(out=sm[:, 0:1], in0=sm[:, 2:3], scalar1=-8.0, scalar2=None, op0=ALU.mult), reads=[smb], writes=[smb])
        op("act", lambda e: e.activation(out=sm[:, 8:16], in_=pcol[:, 38:46], func=AF.Exp, bias=sm[:, 0:1], scale=1.0), reads=[smb, pcolb], writes=[smb])
        op("dve", lambda e: e.tensor_scalar(out=sm[:, 20:24], in0=pcol[:, 34:38], scalar1=-1.0, scalar2=None, op0=ALU.mult), reads=[pcolb, smb], writes=[smb])
        negsmax = sm[:, 0:1]
        sinkexp = sm[:, 8:16]

        off[0] = base
        qaT, _ = al(4 * T, BF16, "qaT")
        qaT3 = qaT.rearrange("p (j t) -> p j t", t=T)
        qab = [[Buf("qa%d_%d" % (j, q)) for q in range(NQ)] for j in range(4)]
        kaT, _ = al(2 * (T + 128), BF16, "kaT")
        kaT3 = kaT.rearrange("p (v t) -> p v t", t=T + 128)
        kab = [[Buf("ka%d_%d" % (kv, q)) for q in range(NQ + 1)] for kv in range(2)]
        vaug, _ = al((NT + 1) * 130, BF16, "vaug")
        vaug4 = vaug.rearrange("p (n v e) -> p n v e", v=2, e=65)
        vab = [Buf("va%d" % n) for n in range(NT + 1)]
        for row in qab:
            for bb in row:
                self.arena_hist.append((base, base + 4 * T, bb))
        for row in kab:
            for bb in row:
                self.arena_hist.append((base + 4 * T, base + 4 * T + 2 * (T + 128), bb))
        for bb in vab:
            self.arena_hist.append((base + 4 * T + 2 * (T + 128), base + 4 * T + 2 * (T + 128) + (NT + 1) * 130, bb))
        mask4, mask4b = al(512, BF16, "mask4")
        def aln(n, dt, name, cnt_):
            r = [al(n, dt, "%s%d" % (name, i)) for i in range(cnt_)]
            return [x[0] for x in r], [x[1] for x in r]
        QW = 3
        qk_tmp_off = off[0]
        qraw, qrawb = aln(512, F32, "qraw", QW)
        sq, sqb = aln(512, BF16, "sqq", QW)
        rstd, rstdb = aln(512, F32, "rstdq", QW)
        qn, qnb = aln(512, BF16, "qn", QW)
        t1, t1b = aln(512, F32, "t1", QW)
        t2, t2b = aln(512, F32, "t2", QW)
        BW = 4
        off_after_qk = off[0]
        off[0] = qk_tmp_off
        Pt = [aln(512, BF16, "P%d_" % par, 2 * BW) for par in range(2)]
        oat, oatb = aln(512, BF16, "oat", BW)
        den, denb = aln(8, F32, "den", BW)
        off[0] = max(off[0], off_after_qk)

        for c in range(4):
            src = self.mprev if c < 2 else self.mcur
            op("pool", lambda e, c=c, src=src: e.tensor_copy(out=mask4[:, c * 128:(c + 1) * 128], in_=src), reads=[cmb], writes=[mask4b])

        ksrc = d["kh_in"] if first else d["kh_out"]
        vsrc = d["vh_in"] if first else d["vh_out"]
        op("pool", lambda e: e.dma_start(out=kaT3[:, :, 0:128], in_=ksrc[l].rearrange("v p t -> p v t")), reads=[self.khb[l]], writes=[kab[0][0], kab[1][0]], dma=True, semkey="khalo")
        op("pool", lambda e: e.dma_start(out=vaug[:, 0:130], in_=vsrc[l]), reads=[self.vhb[l]], writes=[vab[0]], dma=True, semkey="vhalo")
        op("pool", lambda e: e.memset(vaug4[:, 1:, :, 64:65], 1.0), writes=vab[1:])


        cnt = [0]

        def qk_chunk(lhsT_fn, wb, gaincol, dst, dstb, q):
            i = cnt[0] % QW
            cnt[0] += 1
            qs = slice(q * 512, (q + 1) * 512)
            pt, pb = self.psum()
            for k in range(8):
                op("pe", lambda e, pt=pt, k=k: e.matmul(pt[:], lhsT=lhsT_fn(k), rhs=hT[:, k, qs], start=(k == 0), stop=(k == 7)), reads=[wb, hb[q]], writes=[pb])
            op("act", lambda e, pt=pt: e.activation(out=qraw[i], in_=pt[:], func=AF.Copy), reads=[pb], writes=[qrawb[i]])
            op("dve", lambda e: e.tensor_tensor(out=sq[i], in0=qraw[i], in1=qraw[i], op=ALU.mult), reads=[qrawb[i]], writes=[sqb[i]])
            yield
            p2, p2b = self.psum()
            op("pe", lambda e, p2=p2: e.matmul(p2[:], lhsT=self.blk, rhs=sq[i], start=True, stop=True), reads=[sqb[i], cmb], writes=[p2b])
            op("act", lambda e, p2=p2: e.activation(out=rstd[i], in_=p2[:], func=AF.Ln, scale=1.0 / 64, bias=EPS), reads=[p2b], writes=[rstdb[i]])
            op("act", lambda e: e.activation(out=rstd[i], in_=rstd[i], func=AF.Exp, scale=-0.5), reads=[rstdb[i]], writes=[rstdb[i]])
            op("dve", lambda e: e.scalar_tensor_tensor(out=qn[i], in0=qraw[i], scalar=pcol[:, gaincol:gaincol + 1], in1=rstd[i], op0=ALU.mult, op1=ALU.mult),
               reads=[qrawb[i], rstdb[i], pcolb], writes=[qnb[i]])
            yield
            p3, p3b = self.psum()
            op("pe", lambda e, p3=p3: e.matmul(p3[:], lhsT=self.rot, rhs=qn[i], start=True, stop=True), reads=[qnb[i], cmb], writes=[p3b])
            op("dve", lambda e, p3=p3: e.tensor_tensor(out=t2[i], in0=p3[:], in1=self.rope[:, 1, qs], op=ALU.mult), reads=[p3b, self.ropeb], writes=[t2b[i]])
            op("pool", lambda e: e.tensor_tensor(out=t1[i], in0=qn[i], in1=self.rope[:, 0, qs], op=ALU.mult), reads=[qnb[i], self.ropeb], writes=[t1b[i]])
            op("pool", lambda e: e.tensor_tensor(out=dst, in0=t1[i], in1=t2[i], op=ALU.add), reads=[t1b[i], t2b[i]], writes=[dstb])

        def qk_gens():
            for q in range(NQ):
                qs = slice(q * 512, (q + 1) * 512)
                for kv in range(2):
                    yield qk_chunk(lambda k, kv=kv: wka[:, k, kv * 128:(kv + 1) * 128], wkvb, 33, kaT3[:, kv, 128 + q * 512:128 + (q + 1) * 512], kab[kv][q + 1], q)
                for j in range(4):
                    yield qk_chunk(lambda k, j=j: wqa[:, k, j * 128:(j + 1) * 128], wqab, 32, qaT3[:, j, qs], qab[j][q], q)
        run_interleaved(qk_gens(), QW)
        for n in range(NT):
            pt, pb = self.psum()
            for k in range(8):
                op("pe", lambda e, pt=pt, k=k, n=n: e.matmul(pt[:, 0:128], lhsT=hT[:, k, n * 128:(n + 1) * 128], rhs=wva[:, k, :], start=(k == 0), stop=(k == 7)),
                   reads=[hb[n // 4], wkvb], writes=[pb])
            op("act", lambda e, pt=pt, n=n: e.activation(out=vaug4[:, n + 1, :, 0:64], in_=pt[:, 0:128].rearrange("p (a b) -> p a b", b=64), func=AF.Copy),
               reads=[pb], writes=[vab[n + 1]])

        o1 = op("pool", lambda e: e.dma_start(out=d["kh_out"][l].rearrange("v p t -> p v t"), in_=kaT3[:, :, T:T + 128]),
                reads=[kab[0][NQ], kab[1][NQ], kab[0][0], kab[1][0]], writes=[self.khb[l]], dma=True)
        o2 = op("pool", lambda e: e.dma_start(out=d["vh_out"][l], in_=vaug[:, NT * 130:(NT + 1) * 130]), reads=[vab[NT], vab[0]], writes=[self.vhb[l]], dma=True)
        self.outs += [o1, o2]

        def swa_block(i, slot):
            q = i // 4
            for kv in range(2):
                Pv = [Pt[par][0][slot * 2 + kv] for par in range(2)]
                Pb = [Pt[par][1][slot * 2 + kv] for par in range(2)]
                for par in range(2):
                    bank, bankb = self.psum()
                    ps_ = slice(par * 64, (par + 1) * 64)
                    for kt in range(2):
                        kq = (i * 128 + kt * 128) // 512 if (i + kt) > 0 else 0
                        kcol = i * 128 + kt * 128
                        kbuf = kab[kv][0] if (i == 0 and kt == 0) else kab[kv][1 + (kcol - 128) // 512]
                        for jj in range(2):
                            j = 2 * kv + jj
                            col = (kt * 2 + jj) * 128
                            op("pe", lambda e, bank=bank, col=col, ps_=ps_, kcol=kcol, j=j, kv=kv, i=i:
                               e.matmul(bank[:, col:col + 128], lhsT=kaT3[ps_, kv, kcol:kcol + 128], rhs=qaT3[ps_, j, i * 128:(i + 1) * 128], start=True, stop=True),
                               reads=[kbuf, qab[j][q]], writes=[bankb])
                    op("act", lambda e, bank=bank, par=par, Pv=Pv: e.activation(out=Pv[par], in_=bank[:], func=AF.Exp, bias=negsmax, scale=0.125),
                       reads=[bankb, smb], writes=[Pb[par]])
                    op("dve", lambda e, par=par, Pv=Pv: e.tensor_tensor(out=Pv[par], in0=Pv[par], in1=mask4, op=ALU.mult), reads=[Pb[par], mask4b], writes=[Pb[par]])
                yield
                ob_, obb_ = self.psum()
                for par in range(2):
                    for jj in range(2):
                        hl = 2 * jj + par
                        for kt in range(2):
                            col = (kt * 2 + jj) * 128
                            op("pe", lambda e, ob_=ob_, hl=hl, par=par, col=col, kt=kt, kv=kv, i=i, Pv=Pv:
                               e.matmul(ob_[:, hl * 65:(hl + 1) * 65], lhsT=Pv[par][:, col:col + 128], rhs=vaug4[:, i + kt, kv, :], start=(kt == 0), stop=(kt == 1)),
                               reads=[Pb[par], vab[i + kt]], writes=[obb_])
                ii = slot
                ob3 = ob_[:, 0:260].rearrange("p (h e) -> p h e", e=65)
                op("dve", lambda e, ob3=ob3, kv=kv, ii=ii: e.tensor_tensor(out=den[ii][:, kv * 4:(kv + 1) * 4], in0=ob3[:, :, 64], in1=sinkexp[:, kv * 4:(kv + 1) * 4], op=ALU.add),
                   reads=[obb_, smb], writes=[denb[ii]])
                op("dve", lambda e, kv=kv, ii=ii: e.reciprocal(out=den[ii][:, kv * 4:(kv + 1) * 4], in_=den[ii][:, kv * 4:(kv + 1) * 4]), reads=[denb[ii]], writes=[denb[ii]])
                op("dve", lambda e, ob3=ob3, kv=kv, ii=ii: e.tensor_tensor(out=oat[ii][:, kv * 256:(kv + 1) * 256].rearrange("p (h e) -> p h e", e=64), in0=ob3[:, :, 0:64],
                                                                    in1=den[ii][:, kv * 4:(kv + 1) * 4].unsqueeze(2).to_broadcast([128, 4, 64]), op=ALU.mult),
                   reads=[obb_, denb[ii]], writes=[oatb[ii]])
            yield
            ii = slot
            ptr, ptrb = self.psum()
            ptr_bf = ptr[:].bitcast(BF16)
            for j in range(4):
                op("pe", lambda e, ptr_bf=ptr_bf, j=j, ii=ii: e.transpose(out=ptr_bf[:, j * 128:(j + 1) * 128], in_=oat[ii][:, j * 128:(j + 1) * 128], identity=self.ident),
                   reads=[oatb[ii], cmb], writes=[ptrb])
            op("act", lambda e, ptr_bf=ptr_bf, i=i: e.activation(out=oaT3[:, :, i * 128:(i + 1) * 128], in_=ptr_bf[:, 0:512].rearrange("p (j t) -> p j t", t=128), func=AF.Copy),
               reads=[ptrb], writes=[oab[q]])

        run_interleaved((swa_block(i, i % BW) for i in range(NT)), BW)

        off[0] = base
        glrT, glrTb = al(T, BF16, "glrT")
        HS = []
        for sl in range(2):
            h_ = {}
            for nm, n_, dt_ in [("A", T, F32), ("B", T, F32), ("C", T, F32), ("esp0", 512, F32), ("esp1", 512, F32), ("qtT", T, BF16), ("ktT", T, BF16),
                                ("khT", T, BF16), ("khat", NT * 128, BF16), ("vb", NT * 256, BF16), ("At0", 128, BF16), ("At1", 128, BF16),
                                ("rs", 512, F32), ("cst", NT, F32), ("dn", NT, F32)]:
                h_[nm], h_[nm + "b"] = al(n_, dt_, "g%d%s" % (sl, nm))
            h_["ograw"] = self.wslot[sl][:, 0:4096].bitcast(F32)
            h_["ograwb"] = self.wbuf[sl]
            h_["sqo"] = self.wslot[5][:, sl * 1024:(sl + 1) * 1024]
            h_["sqob"] = self.wbuf[5]
            HS.append(h_)

        ssrc = d["st_in"] if first else d["st_out"]
        op("sp", lambda e: e.dma_start(out=self.Sst[:], in_=ssrc[l].rearrange("h c v -> c h v")), reads=[self.stb[l]], writes=self.Sb, dma=True)
        for hd in range(4):
            op("act", lambda e, hd=hd: e.activation(out=self.Sbf[:, hd, :], in_=self.Sst[:, hd, :], func=AF.Copy), reads=[self.Sb[hd]], writes=[self.Sbfb[hd]])

        for q in range(NQ):
            qs = slice(q * 512, (q + 1) * 512)
            pt, pb = self.psum()
            for k in range(8):
                op("pe", lambda e, pt=pt, k=k, qs=qs: e.matmul(pt[0:16, :], lhsT=wglr[:, k, 0:16], rhs=hT[:, k, qs], start=(k == 0), stop=(k == 7)), reads=[wglrb, hb[q]], writes=[pb])
            op("act", lambda e, pt=pt, qs=qs: e.activation(out=glrT[0:16, qs], in_=pt[0:16, :], func=AF.Copy), reads=[pb], writes=[glrTb])

        def gla_head(hd, sl):
            H = HS[sl]
            sp, spb, csum, csumb, Ei, Eib = H["A"], H["Ab"], H["B"], H["Bb"], H["C"], H["Cb"]
            Ee, Eeb, bpos, bposb = sp, spb, csum, csumb
            esp, espb = [H["esp0"], H["esp1"]], [H["esp0b"], H["esp1b"]]
            qtT, qtTb, ktT, ktTb, khT, khTb = H["qtT"], H["qtTb"], H["ktT"], H["ktTb"], H["khT"], H["khTb"]
            khat3 = H["khat"].rearrange("p (n c) -> p n c", c=128)
            khatb = H["khatb"]
            vb3 = H["vb"].rearrange("p (n c) -> p n c", c=256)
            vbb = H["vbb"]
            ograw3 = H["ograw"].rearrange("p (a t) -> p a t", t=T)
            ograwb = H["ograwb"]
            At, Atb = [H["At0"], H["At1"]], [H["At0b"], H["At1b"]]
            sqo3 = H["sqo"].rearrange("p (a t) -> p a t", t=512)
            sqob, rs, rsb, cst, cstb, dn, dnb = H["sqob"], H["rs"], H["rsb"], H["cst"], H["cstb"], H["dn"], H["dnb"]
            csum3 = csum.rearrange("p (n t) -> p n t", t=128)
            srcs = []
            for (c0, w, dc0) in [(C_QB + hd * 128, 128, 0), (C_KB + hd * 128, 128, 128), (C_VB + hd * 256, 256, 256)]:
                src = d["w_in"][l, :, c0:c0 + w].rearrange("(k p) c -> p k c", p=128)
                srcs.append((lambda t, dc0=dc0, w=w: t[:, 0:4096].rearrange("p (k c) -> p k c", c=512)[:, :, dc0:dc0 + w], src))
            th, whb = self.wload(srcs, 3 + sl)
            wh = th[:, 0:4096].rearrange("p (k c) -> p k c", c=512)
            for q in range(NQ):
                qs = slice(q * 512, (q + 1) * 512)
                pz, pzb = self.psum()
                op("pe", lambda e, pz=pz, qs=qs: e.matmul(pz[:], lhsT=self.wgate[0:16, hd * 128:(hd + 1) * 128], rhs=glrT[0:16, qs], start=True, stop=True),
                   reads=[self.wgateb, glrTb], writes=[pzb])
                op("act", lambda e, pz=pz, q=q: e.activation(out=esp[q % 2], in_=pz[:], func=AF.Exp, scale=-1.0, bias=sm[:, 20 + hd:21 + hd]), reads=[pzb, smb], writes=[espb[q % 2]])
                op("act", lambda e, q=q, qs=qs: e.activation(out=sp[:, qs], in_=esp[q % 2], func=AF.Ln, bias=1.0), reads=[espb[q % 2]], writes=[spb])
            yield
            for q in range(NQ):
                qs = slice(q * 512, (q + 1) * 512)
                init = 0.0 if q == 0 else csum[:, q * 512 - 1:q * 512]
                op("dve", lambda e, qs=qs, init=init: e.tensor_tensor_scan(out=csum[:, qs], data0=self.onesf[:, 0:512], data1=sp[:, qs], initial=init, op0=ALU.mult, op1=ALU.add),
                   reads=[spb, self.onesb, csumb], writes=[csumb])
            op("dve", lambda e: e.memset(cst[:, 0:1], 0.0), writes=[cstb])
            op("dve", lambda e: e.tensor_copy(out=cst[:, 1:NT], in_=csum[:, 127:T - 1:128]), reads=[csumb], writes=[cstb])
            op("dve", lambda e: e.tensor_tensor(out=csum3, in0=csum3, in1=cst[:, 0:NT].unsqueeze(2).to_broadcast([128, NT, 128]), op=ALU.subtract), reads=[csumb, cstb], writes=[bposb])
            op("act", lambda e: e.activation(out=Ee, in_=bpos, func=AF.Exp, scale=-1.0 / 16), reads=[bposb, spb], writes=[Eeb])
            op("act", lambda e: e.activation(out=Ei, in_=bpos, func=AF.Exp, scale=1.0 / 16), reads=[bposb], writes=[Eib])
            op("dve", lambda e: e.tensor_copy(out=dn[:, 0:NT], in_=Ee[:, 127:T:128]), reads=[Eeb], writes=[dnb])
            yield
            for q in range(NQ):
                qs = slice(q * 512, (q + 1) * 512)
                pq, pqb = self.psum()
                for k in range(8):
                    op("pe", lambda e, pq=pq, k=k, qs=qs: e.matmul(pq[:], lhsT=wh[:, k, 0:128], rhs=hT[:, k, qs], start=(k == 0), stop=(k == 7)), reads=[whb, hb[q]], writes=[pqb])
                op("dve", lambda e, pq=pq, qs=qs: e.scalar_tensor_tensor(out=qtT[:, qs], in0=pq[:], scalar=float(128 ** -0.5), in1=Ee[:, qs], op0=ALU.mult, op1=ALU.mult),
                   reads=[pqb, Eeb], writes=[qtTb])
                pk, pkb = self.psum()
                for k in range(8):
                    op("pe", lambda e, pk=pk, k=k, qs=qs: e.matmul(pk[:], lhsT=wh[:, k, 128:256], rhs=hT[:, k, qs], start=(k == 0), stop=(k == 7)), reads=[whb, hb[q]], writes=[pkb])
                op("dve", lambda e, pk=pk, qs=qs: e.tensor_tensor(out=ktT[:, qs], in0=pk[:], in1=Ei[:, qs], op=ALU.mult), reads=[pkb, Eib], writes=[ktTb])
            op("dve", lambda e: e.tensor_tensor(out=khT.rearrange("p (n t) -> p n t", t=128), in0=ktT.rearrange("p (n t) -> p n t", t=128),
                                                in1=dn[:, 0:NT].unsqueeze(2).to_broadcast([128, NT, 128]), op=ALU.mult), reads=[ktTb, dnb], writes=[khTb])
            yield
            for n in range(NT):
                pv, pvb = self.psum()
                for k in range(8):
                    op("pe", lambda e, pv=pv, k=k, n=n: e.matmul(pv[:, 0:256], lhsT=hT[:, k, n * 128:(n + 1) * 128], rhs=wh[:, k, 256:512], start=(k == 0), stop=(k == 7)),
                       reads=[whb, hb[n // 4]], writes=[pvb])
                op("act", lambda e, pv=pv, n=n: e.activation(out=vb3[:, n, :], in_=pv[:, 0:256], func=AF.Copy), reads=[pvb], writes=[vbb])
                if n % 4 == 3:
                    yield
            for n0 in range(0, NT, 4):
                ptr, ptrb = self.psum()
                ptr_bf = ptr[:].bitcast(BF16)
                for nn in range(4):
                    n = n0 + nn
                    op("pe", lambda e, ptr_bf=ptr_bf, nn=nn, n=n: e.transpose(out=ptr_bf[:, nn * 128:(nn + 1) * 128], in_=khT[:, n * 128:(n + 1) * 128], identity=self.ident),
                       reads=[khTb, cmb], writes=[ptrb])
                op("act", lambda e, ptr_bf=ptr_bf, n0=n0: e.activation(out=khat3[:, n0:n0 + 4, :], in_=ptr_bf[:, 0:512].rearrange("p (n c) -> p n c", c=128), func=AF.Copy),
                   reads=[ptrb], writes=[khatb])
            yield
            for n in range(NT):
                ns = slice(n * 128, (n + 1) * 128)
                ai = n % 2
                pA, pAb = self.psum()
                op("pe", lambda e, pA=pA, ns=ns: e.matmul(pA[:, 0:128], lhsT=ktT[:, ns], rhs=qtT[:, ns], start=True, stop=True), reads=[ktTb, qtTb], writes=[pAb])
                op("dve", lambda e, pA=pA, ai=ai: e.tensor_tensor(out=At[ai], in0=pA[:, 0:128], in1=self.mcur, op=ALU.mult), reads=[pAb, cmb], writes=[Atb[ai]])
                pU, pUb = self.psum()
                op("pe", lambda e, pU=pU, n=n: e.matmul(pU[:, 0:256], lhsT=khat3[:, n, :], rhs=vb3[:, n, :], start=True, stop=True), reads=[khatb, vbb], writes=[pUb])
                yield
                po, pob = self.psum()
                for vc in range(2):
                    op("pe", lambda e, po=po, vc=vc, n=n, ai=ai: e.matmul(po[:, vc * 128:(vc + 1) * 128], lhsT=vb3[:, n, vc * 128:(vc + 1) * 128], rhs=At[ai], start=True, stop=False),
                       reads=[vbb, Atb[ai]], writes=[pob])
                    op("pe", lambda e, po=po, vc=vc, ns=ns: e.matmul(po[:, vc * 128:(vc + 1) * 128], lhsT=self.Sbf[:, hd, vc * 128:(vc + 1) * 128], rhs=qtT[:, ns], start=False, stop=True),
                       reads=[self.Sbfb[hd], qtTb], writes=[pob])
                op("act", lambda e, po=po, ns=ns: e.activation(out=ograw3[:, :, ns], in_=po[:, 0:256].rearrange("p (a t) -> p a t", t=128), func=AF.Copy), reads=[pob], writes=[ograwb])
                op("dve", lambda e, pU=pU, n=n: e.scalar_tensor_tensor(out=self.Sst[:, hd, :], in0=self.Sst[:, hd, :], scalar=dn[:, n:n + 1], in1=pU[:, 0:256], op0=ALU.mult, op1=ALU.add),
                   reads=[pUb, dnb, self.Sb[hd]], writes=[self.Sb[hd]])
                op("act", lambda e: e.activation(out=self.Sbf[:, hd, :], in_=self.Sst[:, hd, :], func=AF.Copy), reads=[self.Sb[hd]], writes=[self.Sbfb[hd]])
                yield
            for q in range(NQ):
                qs = slice(q * 512, (q + 1) * 512)
                op("pool", lambda e, qs=qs: e.tensor_tensor(out=sqo3, in0=ograw3[:, :, qs], in1=ograw3[:, :, qs], op=ALU.mult), reads=[ograwb], writes=[sqob])
                yield
                pss, pssb = self.psum()
                for vc in range(2):
                    op("pe", lambda e, pss=pss, vc=vc: e.matmul(pss[:], lhsT=self.ones, rhs=sqo3[:, vc, :], start=(vc == 0), stop=(vc == 1)), reads=[sqob, cmb], writes=[pssb])
                op("act", lambda e, pss=pss: e.activation(out=rs, in_=pss[:], func=AF.Ln, scale=1.0 / 256, bias=EPS), reads=[pssb], writes=[rsb])
                op("act", lambda e: e.activation(out=rs, in_=rs, func=AF.Exp, scale=-0.5), reads=[rsb], writes=[rsb])
                for vc in range(2):
                    j = hd * 2 + vc
                    op("dve", lambda e, vc=vc, j=j, qs=qs: e.scalar_tensor_tensor(out=obT3[:, j, qs], in0=ograw3[:, vc, qs], scalar=pcol[:, 24 + j:25 + j], in1=rs, op0=ALU.mult, op1=ALU.mult),
                       reads=[ograwb, rsb, pcolb], writes=[obb[j][q]])
                yield

        run_interleaved((gla_head(hd, hd % 2) for hd in range(4)), 2)
        o3 = op("sp", lambda e: e.dma_start(out=d["st_out"][l].rearrange("h c v -> c h v"), in_=self.Sst[:]), reads=self.Sb, writes=[self.stb[l]], dma=True)
        self.outs.append(o3)

        off[0] = base
        sr, srb = al2(512, BF16, "sr")
        wpa, wpab = self.wrows("w_proj_a", l, 0, 4, D, 2)
        wpb0, wpb0b = self.wrows("w_proj_b", l, 0, 4, D, 3)
        wpb1, wpb1b = self.wrows("w_proj_b", l, 512, 4, D, 4)
        ci = 0
        wr_all = [self.wcols("w_in", l, C_RB + half * 512, 512, half) for half in range(2)]
        for half in range(2):
            wr, wrb = wr_all[half]
            for jj in range(4):
                j = half * 4 + jj
                for q in range(NQ):
                    qs = slice(q * 512, (q + 1) * 512)
                    pr, prb = self.psum()
                    for k in range(8):
                        op("pe", lambda e, pr=pr, k=k, jj=jj, qs=qs, wr=wr: e.matmul(pr[:], lhsT=wr[:, k, jj * 128:(jj + 1) * 128], rhs=hT[:, k, qs], start=(k == 0), stop=(k == 7)),
                           reads=[wrb, hb[q]], writes=[prb])
                    c2 = ci % 2
                    ci += 1
                    op("act", lambda e, pr=pr, c2=c2: e.activation(out=sr[c2], in_=pr[:], func=AF.Silu), reads=[prb], writes=[srb[c2]])
                    op("pool", lambda e, j=j, qs=qs, c2=c2: e.tensor_tensor(out=obT3[:, j, qs], in0=obT3[:, j, qs], in1=sr[c2], op=ALU.mult), reads=[srb[c2], obb[j][q]], writes=[obb[j][q]])

        mT, _ = al(8 * T, BF16, "mT")
        mT3 = mT.rearrange("p (j t) -> p j t", t=T)
        mb = [[Buf("m%d_%d" % (j, q)) for q in range(NQ)] for j in range(8)]
        for row in mb:
            for bb in row:
                self.arena_hist.append((off[0] - 8 * T, off[0], bb))
        sga, sgab = al2(512, F32, "sga")
        sgb_, sgbb = al2(512, F32, "sgb")
        m1, m1b = al2(512, F32, "m1")
        m2, m2b = al2(512, F32, "m2")
        wpb = [(wpb0, wpb0b), (wpb1, wpb1b)]
        gslots = [(5, 0), (1, 5)]
        ci = 0
        wga_pre = {0: self.wcols("w_in", l, C_GA, 512, gslots[0][0])}
        wgb_pre = {0: self.wcols("w_in", l, C_GB, 512, gslots[0][1])}
        wga_pre[1] = self.wcols("w_in", l, C_GA + 512, 512, gslots[1][0])
        for half in range(2):
            wga, wgab = wga_pre[half]
            wgb, wgbb = wgb_pre[half] if half in wgb_pre else self.wcols("w_in", l, C_GB + half * 512, 512, gslots[half][1])
            for dcc in range(4):
                dc = half * 4 + dcc
                ds_ = slice(dc * 128, (dc + 1) * 128)
                for q in range(NQ):
                    qs = slice(q * 512, (q + 1) * 512)
                    c2 = ci % 2
                    ci += 1
                    pya, pyab = self.psum()
                    for j in range(4):
                        op("pe", lambda e, pya=pya, j=j, ds_=ds_, qs=qs: e.matmul(pya[:], lhsT=wpa[:, j, ds_], rhs=oaT3[:, j, qs], start=(j == 0), stop=(j == 3)), reads=[wpab, oab[q]], writes=[pyab])
                    pyb, pybb = self.psum()
                    for j in range(8):
                        w_, wb_ = wpb[j // 4]
                        op("pe", lambda e, pyb=pyb, j=j, ds_=ds_, qs=qs, w_=w_: e.matmul(pyb[:], lhsT=w_[:, j % 4, ds_], rhs=obT3[:, j, qs], start=(j == 0), stop=(j == 7)), reads=[wb_, obb[j][q]], writes=[pybb])
                    pga, pgab = self.psum()
                    for k in range(8):
                        op("pe", lambda e, pga=pga, k=k, dcc=dcc, qs=qs, wga=wga: e.matmul(pga[:], lhsT=wga[:, k, dcc * 128:(dcc + 1) * 128], rhs=hT[:, k, qs], start=(k == 0), stop=(k == 7)), reads=[wgab, hb[q]], writes=[pgab])
                    pgb, pgbb = self.psum()
                    for k in range(8):
                        op("pe", lambda e, pgb=pgb, k=k, dcc=dcc, qs=qs, wgb=wgb: e.matmul(pgb[:], lhsT=wgb[:, k, dcc * 128:(dcc + 1) * 128], rhs=hT[:, k, qs], start=(k == 0), stop=(k == 7)), reads=[wgbb, hb[q]], writes=[pgbb])
                    op("act", lambda e, pga=pga, c2=c2: e.activation(out=sga[c2], in_=pga[:], func=AF.Sigmoid), reads=[pgab], writes=[sgab[c2]])
                    op("act", lambda e, pgb=pgb, c2=c2: e.activation(out=sgb_[c2], in_=pgb[:], func=AF.Sigmoid), reads=[pgbb], writes=[sgbb[c2]])
                    op("dve", lambda e, pya=pya, c2=c2: e.tensor_tensor(out=m1[c2], in0=pya[:], in1=sga[c2], op=ALU.mult), reads=[pyab, sgab[c2]], writes=[m1b[c2]])
                    op("dve", lambda e, pyb=pyb, c2=c2: e.tensor_tensor(out=m2[c2], in0=pyb[:], in1=sgb_[c2], op=ALU.mult), reads=[pybb, sgbb[c2]], writes=[m2b[c2]])
                    op("pool", lambda e, dc=dc, qs=qs, c2=c2: e.tensor_tensor(out=mT3[:, dc, qs], in0=m1[c2], in1=m2[c2], op=ALU.add), reads=[m1b[c2], m2b[c2]], writes=[mb[dc][q]])
        wo = [self.wrows("w_out", l, 0, 4, D, 4), self.wrows("w_out", l, 512, 4, D, 5)]
        if self.stop_after is None:
            self.pre2 = self.ffn_prefetch(l, "ffn2")
        for dc in range(8):
            ds_ = slice(dc * 128, (dc + 1) * 128)
            for q in range(NQ):
                qs = slice(q * 512, (q + 1) * 512)
                po, pob = self.psum()
                for j in range(8):
                    w_, wb_ = wo[j // 4]
                    op("pe", lambda e, po=po, j=j, ds_=ds_, qs=qs, w_=w_: e.matmul(po[:], lhsT=w_[:, j % 4, ds_], rhs=mT3[:, j, qs], start=(j == 0), stop=(j == 7)), reads=[wb_, mb[j][q]], writes=[pob])
                op("dve", lambda e, po=po, dc=dc, qs=qs: e.tensor_tensor(out=xT[:, dc, qs], in0=po[:], in1=xT[:, dc, qs], op=ALU.add), reads=[pob, self.xb[dc][q]], writes=[self.xb[dc][q]])
        if self.dbg:
            for j in range(4):
                self.outs.append(op("sp", lambda e, j=j: e.dma_start(out=d["dbg_oa"][j], in_=oaT3[:, j, :]), reads=oab, writes=[Buf("dbgoa")], dma=True))
            for j in range(8):
                self.outs.append(op("sp", lambda e, j=j: e.dma_start(out=d["dbg_ob"][j], in_=obT3[:, j, :]), reads=obb[j], writes=[Buf("dbgob")], dma=True))


def _consts_host():
    cm = np.zeros((128, 6, 128), np.float32)
    cm[:, 0] = np.eye(128)
    cm[:, 1] = 1.0
    for h in range(2):
        cm[h * 64:(h + 1) * 64, 2, h * 64:(h + 1) * 64] = 1.0
    for m in range(128):
        if m % 64 < 32:
            cm[m + 32, 3, m] = -1.0
        else:
            cm[m - 32, 3, m] = 1.0
    kk = np.arange(128)[:, None]
    qq = np.arange(128)[None, :]
    cm[:, 4] = (kk <= qq)
    cm[:, 5] = (kk > qq)
    return cm.reshape(128, 6 * 128)


def _rope_host(pos0, T):
    inv = (10000.0 ** (-np.arange(0, 64, 2, dtype=np.float32) / 64)).astype(np.float32)
    pos = np.arange(pos0, pos0 + T, dtype=np.float32)
    ang = pos[None, :] * inv[:, None]
    c = np.cos(ang).astype(np.float32)
    s = np.sin(ang).astype(np.float32)
    c128 = np.tile(c, (4, 1))
    s128 = np.tile(s, (4, 1))
    return np.stack([c128, s128], 0)


def _pcol_host(inp, l):
    pc = np.zeros((128, NPCOL), np.float32)
    pc[:, 0:8] = inp["ffn1_norm"][l].reshape(8, 128).T
    pc[:, 8:16] = inp["mix_norm"][l].reshape(8, 128).T
    pc[:, 16:24] = inp["ffn2_norm"][l].reshape(8, 128).T
    pc[:, 24:32] = inp["gla_out_norm"][l].reshape(8, 128).T
    pc[:, 32] = np.tile(inp["swa_q_norm"][l], 2)
    pc[:, 33] = np.tile(inp["swa_k_norm"][l], 2)
    pc[:, 34:38] = inp["gla_gate_bias"][l].reshape(4, 128).T
    pc[:, 38:46] = inp["swa_sinks"][l][None, :]
    pr = np.concatenate([inp["swa_q_norm"][l], inp["swa_k_norm"][l]])[None, :].astype(np.float32)
    return pc, pr


WNAMES = ["ffn1_w_gate", "ffn1_w_up", "ffn1_w_down", "ffn2_w_gate", "ffn2_w_up", "ffn2_w_down",
          "w_in", "gla_w_gate", "w_proj_a", "w_proj_b", "w_out"]

T_SEG = 1024
NSEG = SEQ // T_SEG
N_ACTIVE = 2


def _build():
    steps = [(s, l) for s in range(NSEG) for l in range(DEPTH)]
    P = Prog(T_SEG, NSEG, DEPTH, steps)
    return P.build()


def kernel(**inputs):
    inp = {k: np.ascontiguousarray(np.asarray(v, dtype=np.float32)) for k, v in inputs.items()}
    nc = _build()
    pcs, prs = zip(*[_pcol_host(inp, l) for l in range(DEPTH)])
    pcol = np.stack(pcs)
    prow = np.stack(prs)
    cm = _consts_host()
    rope = np.stack([_rope_host(s * T_SEG, T_SEG) for s in range(NSEG)])
    in_maps = []
    for b in range(N_ACTIVE):
        xin = np.ascontiguousarray(inp["x"][b].reshape(NSEG, T_SEG, D).transpose(0, 2, 1))
        m = {"xin": xin, "rope": rope,
             "st_in": np.zeros((DEPTH, 4, 128, 256), np.float32),
             "kh_in": np.zeros((DEPTH, 2, 128, 128), np.float32),
             "vh_in": np.zeros((DEPTH, 128, 130), np.float32),
             "cmat": cm, "pcol": pcol, "prow": prow}
        for n in WNAMES:
            m[n] = inp[n]
        in_maps.append(m)
    res = run_bass_kernel_spmd(nc, in_maps, core_ids=list(range(N_ACTIVE)))
    out = np.empty((B_, SEQ, D), np.float32)
    for b in range(N_ACTIVE):
        xo = np.asarray(res.results[b]["xout"])
        out[b] = xo.transpose(0, 2, 1).reshape(SEQ, D)
    return out
```
